# Optimizing a Trainium2 kernel written in Bass

```python
import math
import jax, jax.numpy as jnp
from jax import lax
import numpy as np

D_MODEL = 1024
BATCH = 8
SEQ = 8192
DEPTH = 2

GRID_W = 64
CTX_LEN = 256

GDN_HEAD_DIM = 128
GDN_WIDTH = D_MODEL // 2
GDN_HEADS = GDN_WIDTH // GDN_HEAD_DIM
GDN_CHUNK = 64
CONV_K = 5
GMLP_WIDTH = D_MODEL - GDN_WIDTH
GMLP_GROUP_DIM = 128
GMLP_GROUPS = GMLP_WIDTH // GMLP_GROUP_DIM
GMLP_CHUNK = 128
GDN_PROJ = 4 * GDN_WIDTH + 4 * GDN_HEADS
PROJ_WIDTH = GDN_PROJ + 2 * GMLP_WIDTH
MIX_WIDTH = GDN_WIDTH + GMLP_WIDTH
D_FF = 256 * ((8 * D_MODEL // 3 + 255) // 256)
N_EXPERTS = 8
TOP_K = 2
D_FF_EXPERT = 7 * D_MODEL // 2
MOE_BLOCK = 512
N_DENSE = (DEPTH + 1) // 2
N_MOE = DEPTH // 2
EPS = 1e-6

kernel_name = "hybrid_gdn_gmlp_moe_dit"

F32 = jnp.float32


def rms_norm(x, g):
    xf = x.astype(F32)
    y = xf * lax.rsqrt(jnp.mean(xf * xf, axis=-1, keepdims=True) + EPS)
    return (y * g.astype(F32)).astype(x.dtype)


def layer_norm(x, g):
    xf = x.astype(F32)
    mu = jnp.mean(xf, axis=-1, keepdims=True)
    xc = xf - mu
    y = xc * lax.rsqrt(jnp.mean(xc * xc, axis=-1, keepdims=True) + EPS)
    return (y * g.astype(F32)).astype(x.dtype)


def l2_normalize(x):
    return x * lax.rsqrt(jnp.sum(x * x, axis=-1, keepdims=True) + EPS)


def modulate(h, shift, scale):
    return h * (1 + scale) + shift


def grid_pos_embed(rows, dim):
    row = jnp.broadcast_to(jnp.arange(rows, dtype=F32)[:, None], (rows, GRID_W)).reshape(-1)
    col = jnp.broadcast_to(jnp.arange(GRID_W, dtype=F32)[None, :], (rows, GRID_W)).reshape(-1)
    quarter = dim // 4
    omega = 1.0 / (10000.0 ** (jnp.arange(quarter, dtype=F32) / quarter))

    def enc(p):
        ang = p[:, None] * omega[None, :]
        return jnp.concatenate([jnp.sin(ang), jnp.cos(ang)], axis=-1)

    return jnp.concatenate([enc(row), enc(col)], axis=-1)


def short_conv(x, w):
    ch = x.shape[-1]
    y = lax.conv_general_dilated(
        x, w[:, None, :].astype(x.dtype), window_strides=(1,),
        padding=[(CONV_K // 2, CONV_K // 2)],
        dimension_numbers=("NWC", "WIO", "NWC"), feature_group_count=ch)
    return jax.nn.silu(y)


def gdn_chunk_scan(q, k, v, g, beta, s0):
    bsz, seq, nh, dh = q.shape
    n = seq // GDN_CHUNK
    c = GDN_CHUNK

    def to_chunks(t):
        t = t.reshape((bsz, n, c, nh) + t.shape[3:])
        return jnp.moveaxis(jnp.moveaxis(t, 3, 2), 1, 0)

    q = to_chunks(q) * (dh ** -0.5)
    k = to_chunks(k)
    v = to_chunks(v)
    beta = to_chunks(beta)
    gc = jnp.cumsum(to_chunks(g), axis=-1)
    incl = jnp.tril(jnp.ones((c, c), bool))
    strict = jnp.tril(jnp.ones((c, c), bool), -1)
    diff = gc[..., :, None] - gc[..., None, :]
    gamma = jnp.where(incl, jnp.exp(jnp.where(incl, diff, 0.0)), 0.0)
    kb = k * beta[..., None]
    a = jnp.where(strict, jnp.einsum("nbhik,nbhjk->nbhij", kb, k) * gamma, 0.0)
    eye = jnp.eye(c, dtype=a.dtype)
    t_inv = lax.linalg.triangular_solve(eye + a, jnp.broadcast_to(eye, a.shape),
                                        left_side=True, lower=True, unit_diagonal=True)
    u = jnp.einsum("nbhij,nbhjd->nbhid", t_inv, v * beta[..., None])
    w = jnp.einsum("nbhij,nbhjd->nbhid", t_inv, kb * jnp.exp(gc)[..., None])
    qk = jnp.where(incl, jnp.einsum("nbhik,nbhjk->nbhij", q, k) * gamma, 0.0)
    q_dec = q * jnp.exp(gc)[..., None]
    k_dec = k * jnp.exp(gc[..., -1:] - gc)[..., None]
    g_last = jnp.exp(gc[..., -1])

    def step(s, inp):
        q_c, k_c, u_c, w_c, qk_c, gl = inp
        v_new = u_c - jnp.einsum("bhck,bhkv->bhcv", w_c, s)
        o = jnp.einsum("bhck,bhkv->bhcv", q_c, s) + jnp.einsum("bhij,bhjv->bhiv", qk_c, v_new)
        s = s * gl[..., None, None] + jnp.einsum("bhck,bhcv->bhkv", k_c, v_new)
        return s, o

    s, o = lax.scan(step, s0, (q_dec, k_dec, u, w, qk, g_last))
    o = jnp.moveaxis(jnp.moveaxis(o, 0, 1), 2, 3).reshape(bsz, seq, nh, dh)
    return o, s


def gdn_branch(p, conv_w, a_log, dt_bias, o_norm_g, s_f0, s_b0):
    bsz, seq, _ = p.shape
    qkv = short_conv(p[..., :3 * GDN_WIDTH], conv_w).astype(F32)
    qkv = qkv.reshape(bsz, seq, 3, GDN_HEADS, GDN_HEAD_DIM)
    q = l2_normalize(qkv[:, :, 0])
    k = l2_normalize(qkv[:, :, 1])
    v = qkv[:, :, 2]
    z = p[..., 3 * GDN_WIDTH:4 * GDN_WIDTH].reshape(bsz, seq, GDN_HEADS, GDN_HEAD_DIM)
    gates = p[..., 4 * GDN_WIDTH:GDN_PROJ].astype(F32).reshape(bsz, seq, 2, 2, GDN_HEADS)
    decay = -jnp.exp(a_log.astype(F32)) * jax.nn.softplus(gates[:, :, :, 0] + dt_bias.astype(F32))
    beta = jax.nn.sigmoid(gates[:, :, :, 1])
    o_f, s_f = gdn_chunk_scan(q, k, v, decay[:, :, 0], beta[:, :, 0], s_f0)
    rev = lambda t: jnp.flip(t, axis=1)
    o_b, s_b = gdn_chunk_scan(rev(q), rev(k), rev(v), rev(decay[:, :, 1]), rev(beta[:, :, 1]), s_b0)
    o = o_f + rev(o_b)
    o = rms_norm(o, o_norm_g) * jax.nn.silu(z.astype(F32))
    return o.reshape(bsz, seq, GDN_WIDTH).astype(p.dtype), s_f, s_b


def gmlp_branch(p, sgu_norm_g, w_s, b_s):
    bsz, seq, _ = p.shape
    gu = jax.nn.gelu(p[..., GDN_PROJ:GDN_PROJ + GMLP_WIDTH])
    gv = layer_norm(jax.nn.gelu(p[..., GDN_PROJ + GMLP_WIDTH:]), sgu_norm_g)
    gv = gv.reshape(bsz, seq // GMLP_CHUNK, GMLP_CHUNK, GMLP_GROUPS, GMLP_GROUP_DIM)
    mixed = jnp.einsum("gpq,bnqgc->bnpgc", w_s, gv) + jnp.swapaxes(b_s, 0, 1)[None, None, :, :, None]
    return gu * mixed.reshape(bsz, seq, GMLP_WIDTH)


def swiglu(h, w1, w3, w2):
    return (jax.nn.silu(h @ w1) * (h @ w3)) @ w2


def moe_swiglu(h, router_w, router_b, w1, w3, w2):
    n_tok, dim = h.shape
    n_assign = n_tok * TOP_K
    logits = h.astype(F32) @ router_w.astype(F32) + router_b.astype(F32)
    probs = jax.nn.softmax(logits, axis=-1)
    top_p, top_e = lax.top_k(probs, TOP_K)
    top_p = top_p / jnp.sum(top_p, axis=-1, keepdims=True)
    flat_e = top_e.reshape(-1)
    flat_tok = jnp.arange(n_assign, dtype=jnp.int32) // TOP_K
    order = jnp.argsort(flat_e)
    sorted_e = flat_e[order]
    sorted_tok = flat_tok[order]
    sorted_p = top_p.reshape(-1)[order]
    counts = jnp.bincount(flat_e, length=N_EXPERTS)
    padded = (counts + MOE_BLOCK - 1) // MOE_BLOCK * MOE_BLOCK
    seg_end = jnp.cumsum(padded)
    pad_start = seg_end - padded
    start = jnp.cumsum(counts) - counts
    dest = pad_start[sorted_e] + jnp.arange(n_assign, dtype=jnp.int32) - start[sorted_e]
    n_blocks = -(-n_assign // MOE_BLOCK) + N_EXPERTS
    buf_tok = jnp.zeros((n_blocks * MOE_BLOCK,), jnp.int32).at[dest].set(sorted_tok)
    block_e = jnp.minimum(
        jnp.searchsorted(seg_end, jnp.arange(n_blocks, dtype=jnp.int32) * MOE_BLOCK, side="right"),
        N_EXPERTS - 1)
    xb = h[buf_tok].reshape(n_blocks, MOE_BLOCK, dim)

    def expert_block(args):
        xe, e = args
        return swiglu(xe, w1[e], w3[e], w2[e])

    yb = lax.map(expert_block, (xb, block_e)).reshape(-1, dim)
    y = yb[dest] * sorted_p[:, None].astype(h.dtype)
    return jnp.zeros_like(h).at[sorted_tok].add(y)


def setup_inputs(seed: int = 0) -> dict:
    key = jax.random.key(seed)
    ks = iter(jax.random.split(key, 32))
    nrm = lambda shape, s: jax.random.normal(next(ks), shape, F32) * s
    gain = lambda shape: 1.0 + nrm(shape, 0.02)
    d = D_MODEL
    a_init = jax.random.uniform(next(ks), (DEPTH, 2, GDN_HEADS), F32, 1.0, 16.0)
    dt = jnp.exp(jax.random.uniform(next(ks), (DEPTH, 2, GDN_HEADS), F32, math.log(1e-3), math.log(1e-1)))
    return {
        "x": nrm((BATCH, SEQ, d), 1.0),
        "c": nrm((BATCH, d), 1.0),
        "ctx": nrm((BATCH, CTX_LEN, d), 1.0),
        "c_ctx": nrm((d,), 1.0),
        "w_mod": nrm((DEPTH, d, 6 * d), 0.5 * d ** -0.5),
        "b_mod": nrm((DEPTH, 6 * d), 0.02),
        "norm1_g": gain((DEPTH, d)),
        "norm2_g": gain((DEPTH, d)),
        "w_in": nrm((DEPTH, d, PROJ_WIDTH), d ** -0.5),
        "conv_w": nrm((DEPTH, CONV_K, 3 * GDN_WIDTH), CONV_K ** -0.5),
        "a_log": jnp.log(a_init),
        "dt_bias": dt + jnp.log(-jnp.expm1(-dt)),
        "o_norm_g": gain((DEPTH, GDN_HEAD_DIM)),
        "sgu_norm_g": gain((DEPTH, GMLP_WIDTH)),
        "w_s": nrm((DEPTH, GMLP_GROUPS, GMLP_CHUNK, GMLP_CHUNK), 0.5 * GMLP_CHUNK ** -0.5),
        "b_s": gain((DEPTH, GMLP_GROUPS, GMLP_CHUNK)),
        "w_out": nrm((DEPTH, MIX_WIDTH, d), MIX_WIDTH ** -0.5),
        "ffn_w1": nrm((N_DENSE, d, D_FF), d ** -0.5),
        "ffn_w3": nrm((N_DENSE, d, D_FF), d ** -0.5),
        "ffn_w2": nrm((N_DENSE, D_FF, d), D_FF ** -0.5),
        "router_w": nrm((N_MOE, d, N_EXPERTS), d ** -0.5),
        "router_b": nrm((N_MOE, N_EXPERTS), 0.01),
        "exp_w1": nrm((N_MOE, N_EXPERTS, d, D_FF_EXPERT), d ** -0.5),
        "exp_w3": nrm((N_MOE, N_EXPERTS, d, D_FF_EXPERT), d ** -0.5),
        "exp_w2": nrm((N_MOE, N_EXPERTS, D_FF_EXPERT, d), D_FF_EXPERT ** -0.5),
        "final_g": gain((d,)),
    }


def reference(x, c, ctx, c_ctx, w_mod, b_mod, norm1_g, norm2_g, w_in, conv_w, a_log, dt_bias,
              o_norm_g, sgu_norm_g, w_s, b_s, w_out, ffn_w1, ffn_w3, ffn_w2, router_w, router_b,
              exp_w1, exp_w3, exp_w2, final_g):
    bsz, seq, dim = x.shape
    rows = seq // GRID_W
    x = x + grid_pos_embed(rows, dim).astype(x.dtype)[None]
    s_c = jax.nn.silu(c)
    s_ctx = jax.nn.silu(c_ctx)
    zero_state = jnp.zeros((bsz, GDN_HEADS, GDN_HEAD_DIM, GDN_HEAD_DIM), F32)

    def channel_mixer(layer, h):
        if layer % 2 == 0:
            i = layer // 2
            return swiglu(h, ffn_w1[i], ffn_w3[i], ffn_w2[i])
        i = layer // 2
        flat = moe_swiglu(h.reshape(-1, dim), router_w[i], router_b[i], exp_w1[i], exp_w3[i], exp_w2[i])
        return flat.reshape(h.shape)

    for layer in range(DEPTH):
        last = layer == DEPTH - 1
        mod = s_c @ w_mod[layer] + b_mod[layer]
        mod_c = s_ctx @ w_mod[layer] + b_mod[layer]
        sh1, sc1, g1, sh2, sc2, g2 = jnp.split(mod[:, None, :], 6, axis=-1)
        csh1, csc1, cg1, csh2, csc2, cg2 = jnp.split(mod_c, 6, axis=-1)

        pc = modulate(rms_norm(ctx, norm1_g[layer]), csh1, csc1) @ w_in[layer]
        oc, s_f, s_b = gdn_branch(pc, conv_w[layer], a_log[layer], dt_bias[layer], o_norm_g[layer],
                                  zero_state, zero_state)
        px = modulate(rms_norm(x, norm1_g[layer]), sh1, sc1) @ w_in[layer]
        ox, _, _ = gdn_branch(px, conv_w[layer], a_log[layer], dt_bias[layer], o_norm_g[layer], s_f, s_b)
        mx = gmlp_branch(px, sgu_norm_g[layer], w_s[layer], b_s[layer])
        x = x + g1 * (jnp.concatenate([ox, mx], axis=-1) @ w_out[layer])
        if not last:
            mc = gmlp_branch(pc, sgu_norm_g[layer], w_s[layer], b_s[layer])
            ctx = ctx + cg1 * (jnp.concatenate([oc, mc], axis=-1) @ w_out[layer])

        hx = modulate(rms_norm(x, norm2_g[layer]), sh2, sc2)
        x = x + g2 * channel_mixer(layer, hx)
        if not last:
            hc = modulate(rms_norm(ctx, norm2_g[layer]), csh2, csc2)
            ctx = ctx + cg2 * channel_mixer(layer, hc)

    return rms_norm(x, final_g)
```

```python
import math
from contextlib import ExitStack
import numpy as np
import concourse.bass as bass
import concourse.mybir as mybir
from concourse.bass_utils import run_bass_kernel_spmd

F32 = mybir.dt.float32
BF16 = mybir.dt.bfloat16
AF = mybir.ActivationFunctionType
ALU = mybir.AluOpType

D = 1024
KC = 8
PROJ = 3088
DFF = 2816
DFE = 3584
NE = 8
EPS = 1e-6
NEG = -30000.0
EPOCH = 30000
NDMA_SEMS = 32
NSW_SEMS = 12


class Prog:
    ENGS = ("pe", "act", "dve", "pool", "sp")

    def __init__(self, nc, stack):
        self.nc = nc
        self.stack = stack
        self.streams = {e: [] for e in self.ENGS}
        self.cnt = {e: 0 for e in self.ENGS}
        self.esems = {e: [] for e in self.ENGS}
        self.seen = {e: {} for e in self.ENGS}
        self.last_w = {}
        self.readers = {}
        self.dma_sems = {"hw": [stack.enter_context(nc.semaphore(f"dq{i}")) for i in range(NDMA_SEMS)],
                         "sw": [stack.enter_context(nc.semaphore(f"dw{i}")) for i in range(NSW_SEMS)]}
        self.ndma = {"hw": 0, "sw": 0}

    def _eng_sem(self, e, epoch):
        while len(self.esems[e]) <= epoch:
            self.esems[e].append(self.stack.enter_context(self.nc.semaphore(f"s_{e}_{len(self.esems[e])}")))
        return self.esems[e][epoch]

    def _need(self, e, tok, waits):
        if tok is None:
            return
        sem, val, src = tok
        if src == e and e == "pe":
            return
        sid = id(sem)
        if self.seen[e].get(sid, 0) >= val:
            return
        self.seen[e][sid] = val
        waits.append((sem, val))

    @staticmethod
    def _x(reads, writes):
        r, w = [], list(writes)
        for k in reads:
            if isinstance(k, tuple) and k and k[0] == "PS":
                w.append(k)
            else:
                r.append(k)
        return r, w

    def _deps(self, e, reads, writes):
        reads, writes = self._x(reads, writes)
        waits = []
        for k in reads:
            self._need(e, self.last_w.get(k), waits)
        for k in writes:
            self._need(e, self.last_w.get(k), waits)
            for t in self.readers.get(k, ()):
                if t[2] == e and t[3] is False:
                    continue
                self._need(e, t[:3], waits)
        return waits

    def _commit(self, tok, reads, writes, is_dma):
        reads, writes = self._x(reads, writes)
        for k in reads:
            self.readers.setdefault(k, []).append(tok + (is_dma,))
        for k in writes:
            self.last_w[k] = tok
            self.readers[k] = []

    def op(self, e, fn, reads=(), writes=()):
        waits = self._deps(e, reads, writes)
        n = self.cnt[e]
        epoch, idx = divmod(n, EPOCH)
        sem = self._eng_sem(e, epoch)
        self.cnt[e] = n + 1
        tok = (sem, idx + 1, e)
        self.streams[e].append((waits, fn, sem, 1))
        self._commit(tok, reads, writes, False)

    def dma(self, e, out, in_, reads=(), writes=()):
        waits = self._deps(e, reads, writes)
        kind = "sw" if e == "pool" else "hw"
        pool_ = self.dma_sems[kind]
        n = self.ndma[kind]
        self.ndma[kind] += 1
        slot, rnd = n % len(pool_), n // len(pool_)
        sem = pool_[slot]
        if rnd > 0:
            self._need(e, (sem, 16 * rnd, None), waits)
        tok = (sem, 16 * (rnd + 1), None)
        self.streams[e].append((waits, lambda eng: eng.dma_start(out=out, in_=in_), sem, 16))
        self._commit(tok, reads, writes, True)
        return tok

    def barrier(self):
        toks = []
        for e in self.ENGS:
            n = self.cnt[e]
            if n:
                epoch, idx = divmod(n - 1, EPOCH)
                toks.append((self.esems[e][epoch], idx + 1, None))
        for kind, pool_ in self.dma_sems.items():
            nd = self.ndma[kind]
            for n in range(max(0, nd - len(pool_)), nd):
                toks.append((pool_[n % len(pool_)], 16 * (n // len(pool_) + 1), None))
        for e in self.ENGS:
            waits = []
            for t in toks:
                self._need(e, t, waits)
            if waits:
                self.streams[e].append((waits, None, None, 0))

    def final_wait(self, e, toks):
        waits = []
        for t in toks:
            self._need(e, t, waits)
        self.streams[e].append((waits, None, None, 0))

    def emit(self):
        nc = self.nc
        with nc.Block() as block:
            def run(eng, stream):
                for waits, fn, sem, inc in stream:
                    for (s, v) in waits:
                        eng.wait_ge(s, v)
                    if fn is not None:
                        fn(eng).then_inc(sem, inc)

            @block.tensor
            def _(eng):
                run(eng, self.streams["pe"])

            @block.scalar
            def _(eng):
                run(eng, self.streams["act"])

            @block.vector
            def _(eng):
                run(eng, self.streams["dve"])

            @block.gpsimd
            def _(eng):
                run(eng, self.streams["pool"])

            @block.sync
            def _(eng):
                run(eng, self.streams["sp"])


def _cmap():
    off = {}
    o = 0

    def add(name, n):
        nonlocal o
        off[name] = (o, n)
        o += n

    add("ident", 128)
    add("Lf", 128)
    add("Lb", 128)
    add("ones", 128)
    for nm in ("nm_f_incl", "nm_f_strict", "nm_b_incl", "nm_b_strict"):
        add(nm, 128)
    add("sel", 1024)
    add("D2", 128)
    for sz in (2, 4, 8, 16, 32, 64):
        add(f"B{sz}", 128)
    for l in range(2):
        add(f"n1g{l}", 8)
        add(f"n2g{l}", 8)
        add(f"bmod{l}", 48)
        add(f"convw{l}", 60)
        add(f"ong{l}", 1)
        add(f"alog{l}", 1)
        add(f"dtb{l}", 1)
        add(f"sgug{l}", 512)
        add(f"bsrow{l}", 512)
    add("fing", 8)
    add("cvec", 16)
    add("rbrow", 8)
    return off, o


CMAP, NCST = _cmap()


def build_consts(inp, b):
    c = np.zeros((128, NCST), np.float32)

    def put(name, arr):
        o, n = CMAP[name]
        arr = np.asarray(arr, np.float32)
        assert arr.shape[1] == n, (name, arr.shape, n)
        c[:arr.shape[0], o:o + n] = arr

    p = np.arange(128)[:, None]
    f = np.arange(128)[None, :]
    put("ident", (p == f))
    put("Lf", (f >= p))
    put("Lb", (f <= p))
    put("ones", np.ones((128, 128)))
    put("nm_f_incl", np.where(f >= p, 0.0, NEG))
    put("nm_f_strict", np.where(f > p, 0.0, NEG))
    put("nm_b_incl", np.where(f <= p, 0.0, NEG))
    put("nm_b_strict", np.where(f < p, 0.0, NEG))
    put("D2", (p // 2 == f // 2))
    for sz in (2, 4, 8, 16, 32, 64):
        put(f"B{sz}", (p // (2 * sz) == f // (2 * sz)) & (p // sz != f // sz))
    sel = np.zeros((8, 8, 128), np.float32)
    for r in range(8):
        sel[r, r, :] = 1.0
    put("sel", sel.reshape(8, 1024))
    fm = lambda v: np.asarray(v, np.float32).reshape(-1, 128).T
    for l in range(2):
        put(f"n1g{l}", fm(inp["norm1_g"][l]))
        put(f"n2g{l}", fm(inp["norm2_g"][l]))
        put(f"bmod{l}", fm(inp["b_mod"][l]))
        cw = np.asarray(inp["conv_w"][l], np.float32)
        put(f"convw{l}", cw.T.reshape(12, 128, 5).transpose(1, 0, 2).reshape(128, 60))
        put(f"ong{l}", np.asarray(inp["o_norm_g"][l], np.float32).reshape(128, 1))
        al = np.zeros((16, 1), np.float32)
        db = np.zeros((16, 1), np.float32)
        for d_ in range(2):
            al[d_ * 8:d_ * 8 + 4, 0] = inp["a_log"][l][d_]
            db[d_ * 8:d_ * 8 + 4, 0] = inp["dt_bias"][l][d_]
        put(f"alog{l}", al)
        put(f"dtb{l}", db)
        put(f"sgug{l}", np.broadcast_to(np.asarray(inp["sgu_norm_g"][l], np.float32)[None, :], (128, 512)))
        put(f"bsrow{l}", np.broadcast_to(np.asarray(inp["b_s"][l], np.float32).reshape(1, 512), (128, 512)))
    put("fing", fm(inp["final_g"]))
    cv = np.stack([fm(inp["c"][b]), fm(inp["c_ctx"])], axis=-1)
    put("cvec", cv.reshape(128, 16))
    put("rbrow", np.broadcast_to(np.asarray(inp["router_b"][0], np.float32)[None, :], (128, 8)))
    return c


def grid_pos_embed_T(seq, dim, grid_w=64):
    rows = seq // grid_w
    row = np.broadcast_to(np.arange(rows, dtype=np.float32)[:, None], (rows, grid_w)).reshape(-1)
    col = np.broadcast_to(np.arange(grid_w, dtype=np.float32)[None, :], (rows, grid_w)).reshape(-1)
    quarter = dim // 4
    omega = (1.0 / (10000.0 ** (np.arange(quarter, dtype=np.float32) / np.float32(quarter)))).astype(np.float32)

    def enc(pv):
        ang = (pv[:, None] * omega[None, :]).astype(np.float32)
        return np.concatenate([np.sin(ang), np.cos(ang)], axis=-1)

    return np.ascontiguousarray(np.concatenate([enc(row), enc(col)], axis=-1).astype(np.float32).T)


def mm(out, lhsT, rhs, start=True, stop=True):
    return lambda e: e.matmul(out, lhsT=lhsT, rhs=rhs, start=start, stop=stop)


def act(out, in_, func, **kw):
    return lambda e: e.activation(out=out, in_=in_, func=func, **kw)


def tt(out, in0, in1, op):
    return lambda e: e.tensor_tensor(out=out, in0=in0, in1=in1, op=op)


def ts(out, in0, s1, op0, s2=None, op1=None):
    if op1 is None:
        return lambda e: e.tensor_scalar(out=out, in0=in0, scalar1=s1, scalar2=None, op0=op0)
    return lambda e: e.tensor_scalar(out=out, in0=in0, scalar1=s1, scalar2=s2, op0=op0, op1=op1)


def stt(out, in0, scalar, in1, op0, op1):
    return lambda e: e.scalar_tensor_tensor(out=out, in0=in0, scalar=scalar, in1=in1, op0=op0, op1=op1)


def cp(out, in_):
    return lambda e: e.tensor_copy(out=out, in_=in_)


def ms(ap, val):
    return lambda e: e.memset(ap, val)


def tr(out, in_, ident):
    return lambda e: e.transpose(out=out, in_=in_, identity=ident)


def red(out, in_, op):
    return lambda e: e.tensor_reduce(out=out, in_=in_, axis=mybir.AxisListType.X, op=op)


class Builder:
    def __init__(self, S, CT, debug=False):
        self.S, self.CT, self.debug = S, CT, debug
        self.NT = min(1024, S)

    def f32(self, name, cols):
        self.alog = getattr(self, "alog", [])
        self.alog.append((name, self.apos, cols, "f32"))
        a = self.arena[:, self.apos:self.apos + cols]
        self.apos += cols
        assert self.apos <= self.AW, (name, self.apos, self.AW)
        return a

    def b16(self, name, cols):
        w = (cols + 1) // 2
        self.alog = getattr(self, "alog", [])
        self.alog.append((name, self.apos, cols, "b16"))
        a = self.arena[:, self.apos:self.apos + w].bitcast(BF16)
        self.apos += w
        assert self.apos <= self.AW, (name, self.apos, self.AW)
        return a[:, 0:cols]

    def cs(self, name, n0=0, n1=None, rows=128):
        o, n = CMAP[name]
        if n1 is None:
            n1 = n
        return self.cst[0:rows, o + n0:o + n1]

    def _bank(self):
        i = self._pb % len(self.banks)
        self._pb += 1
        return self.banks[i], ("PS", i)

    def ps_half(self):
        bk, k = self._bank()
        return bk[:, 0:256], k

    def ps_q(self):
        bk, k = self._bank()
        return bk[:, 0:128], k

    def ps_t(self):
        i = self._pt % len(self.pt16)
        self._pt += 1
        return self.pt16[i][:, 0:128], ("PS", "t", i)

    def ps_full(self, lo=0, n=7):
        bk, k = self._bank()
        return bk[:, :], k

    def build(self):
        S, CT = self.S, self.CT
        nc = bass.Bass("TRN2", target_bir_lowering=False)
        self.nc = nc
        din = lambda name, shape, dt=F32: nc.dram_tensor(name, shape, dt, kind="ExternalInput").ap()
        dscr = lambda name, shape, dt=F32: nc.dram_tensor(
            name, shape, dt, kind="ExternalOutput" if self.debug else "Internal").ap()
        self.xT = din("xT", [D, S])
        self.ctxT = din("ctxT", [D, CT])
        self.posT = din("posT", [D, S])
        self.cst_d = din("cst", [128, NCST])
        self.w_mod = [din(f"w_mod{l}", [D, 6 * D]) for l in range(2)]
        self.w_in = [din(f"w_in{l}", [D, PROJ]) for l in range(2)]
        self.w_out = [din(f"w_out{l}", [D, D]) for l in range(2)]
        self.wsT = din("wsT", [2, 128, 512])
        self.ffn_w1 = din("ffn_w1", [D, DFF])
        self.ffn_w3 = din("ffn_w3", [D, DFF])
        self.ffn_w2 = din("ffn_w2", [DFF, D])
        self.exp_w1 = din("exp_w1", [NE, D, DFE])
        self.exp_w3 = din("exp_w3", [NE, D, DFE])
        self.exp_w2 = din("exp_w2", [NE, DFE, D])
        self.router_w = din("router_w", [D, NE])
        self.outT = nc.dram_tensor("outT", [D, S], F32, kind="ExternalOutput").ap()
        self.xa = dscr("xa", [D, S])
        self.xb = dscr("xb", [D, S])
        self.ca = dscr("ca", [D, CT])
        self.cb = dscr("cb", [D, CT])
        self.of = dscr("of", [512, CT + S])
        self.w_in_b = [dscr(f"w_in_b{l}", [128, KC, PROJ], BF16) for l in range(2)]
        self.w_out_b = [dscr(f"w_out_b{l}", [128, KC, D], BF16) for l in range(2)]
        self.f13b = dscr("f13b", [6, 128, 2, KC, 512], BF16)
        self.f2b = dscr("f2b", [4, 128, 22, 256], BF16)
        self.e13b = dscr("e13b", [NE, 7, 128, 2, KC, 512], BF16)
        self.e2b = dscr("e2b", [NE, 4, 128, 28, 256], BF16)

        with ExitStack() as st:
            self.P = P = Prog(nc, st)
            self.AW = 50500
            self.arena = st.enter_context(nc.sbuf_tensor("arena", [128, self.AW], F32))
            self.banks = [st.enter_context(nc.psum_tensor(f"bank{i}", [128, 512], F32)) for i in range(6)]
            self.pt16 = [st.enter_context(nc.psum_tensor(f"pt16_{i}", [128, 1024], BF16)) for i in range(2)]
            self._pb = self._pt = 0
            self.apos = 0
            self.setup()
            self.persist_end = self.apos
            for l in range(2):
                self.apos = self.persist_end
                P.barrier()
                self.sweep_phase(l)
                self.apos = self.persist_end
                P.barrier()
                self.mlp_phase(l)
            P.barrier()
            P.final_wait("sp", self.out_toks)
            P.emit()
        return nc

    def setup(self):
        P, nc = self.P, self.nc
        S, CT = self.S, self.CT
        self.out_toks = []
        self.cst = self.f32("cst", NCST)
        P.dma("sp", self.cst, self.cst_d, writes=["cst"])
        for l in range(2):
            P.dma("pool", self.w_in_b[l], self.w_in[l].rearrange("(kc p) n -> p kc n", p=128), writes=[("w_in_b", l)])
            P.dma("pool", self.w_out_b[l], self.w_out[l].rearrange("(kc p) n -> p kc n", p=128), writes=[("w_out_b", l)])
        self.ident16 = self.b16("ident16", 128)
        self.ones16 = self.b16("ones16", 128)
        self.nm16 = {}
        P.op("dve", cp(self.ident16, self.cs("ident")), reads=["cst"], writes=["ident16"])
        P.op("dve", ms(self.ones16, 1.0), writes=["ones16"])
        for nm in ("nm_f_incl", "nm_f_strict", "nm_b_incl", "nm_b_strict", "D2", "B2", "B4", "B8", "B16", "B32", "B64"):
            t = self.b16(nm, 128)
            self.nm16[nm] = t
            P.op("dve", cp(t, self.cs(nm)), reads=["cst"], writes=[nm])
        self.wsT16 = self.b16("wsT16", 1024)
        P.dma("pool", self.wsT16.rearrange("p (l n) -> p l n", l=2), self.wsT.rearrange("l q n -> q l n"),
              writes=["wsT16"])
        self.rw32 = self.f32("rw32", 64)
        P.dma("sp", self.rw32.rearrange("p (k n) -> p k n", k=8), self.router_w.rearrange("(k p) n -> p k n", p=128),
              writes=["rw32"])
        s32 = self.f32("s32", 16)
        P.op("act", act(s32, self.cs("cvec"), AF.Silu), reads=["cst"], writes=["s32"])
        s3 = s32.rearrange("p (k s) -> p k s", k=8)
        modvs = [self.f32(f"modv{l}", 96) for l in range(2)]
        self.vec = {}
        for l in range(2):
            for s in range(2):
                self.vec[(l, s)] = {nm: self.f32(nm, 8) for nm in ("gs1", "sh1", "g1", "gs2", "sh2", "g2")}
            self.vec[(l, "nega")] = self.f32("nega", 1)
        keep = self.apos
        wmb = [self.f32(f"wmb{i}", KC * 512).rearrange("p (k n) -> p k n", k=8) for i in range(2)]
        nblk = 0
        for l in range(2):
            modv = modvs[l]
            for blk in range(12):
                wb = wmb[nblk % 2]
                key = ("wmb", nblk % 2)
                nblk += 1
                P.dma("sp", wb, self.w_mod[l].rearrange("(k p) n -> p k n", p=128)[:, :, blk * 512:(blk + 1) * 512],
                      writes=[key])
                for j in range(4):
                    oc = blk * 4 + j
                    pq, pk = self.ps_q()
                    for kc in range(KC):
                        P.op("pe", mm(pq[:, 0:2], wb[:, kc, j * 128:(j + 1) * 128], s3[:, kc, :], kc == 0, kc == KC - 1),
                             reads=[key, "s32"], writes=[pk])
                    P.op("dve", ts(modv[:, oc * 2:oc * 2 + 2], pq[:, 0:2], self.cs(f"bmod{l}", oc, oc + 1), ALU.add),
                         reads=[pk, "cst"], writes=[("modv", l)])
            mv = modv.rearrange("p (o s) -> p o s", s=2)
            for s in range(2):
                v = self.vec[(l, s)]
                for nm, part, ng in (("1", 0, f"n1g{l}"), ("2", 3, f"n2g{l}")):
                    P.op("dve", stt(v["gs" + nm], mv[:, (part + 1) * 8:(part + 2) * 8, s], 1.0, self.cs(ng), ALU.add, ALU.mult),
                         reads=[("modv", l), "cst"], writes=[("vec", l, s)])
                    P.op("dve", cp(v["sh" + nm], mv[:, part * 8:(part + 1) * 8, s]), reads=[("modv", l)], writes=[("vec", l, s)])
                    P.op("dve", cp(v["g" + nm], mv[:, (part + 2) * 8:(part + 3) * 8, s]), reads=[("modv", l)], writes=[("vec", l, s)])
            na = self.vec[(l, "nega")]
            P.op("act", act(na[0:16, :], self.cs(f"alog{l}", rows=16), AF.Exp), reads=["cst"], writes=[("nega", l)])
            P.op("dve", ts(na[0:16, :], na[0:16, :], -1.0, ALU.mult), reads=[("nega", l)], writes=[("nega", l)])
        TT = min(512, S)
        xt = [self.f32("xt0", KC * TT).rearrange("p (k n) -> p k n", k=8) for _ in range(2)]
        pt = [self.f32("pt0", KC * TT).rearrange("p (k n) -> p k n", k=8) for _ in range(2)]
        r3 = lambda ap: ap.rearrange("(k p) t -> p k t", p=128)
        for i in range(S // TT):
            b = i % 2
            sl = slice(i * TT, (i + 1) * TT)
            P.dma("sp", xt[b], r3(self.xT)[:, :, sl], writes=[("x0", b)])
            P.dma("sp", pt[b], r3(self.posT)[:, :, sl], writes=[("p0", b)])
            P.op("dve", tt(xt[b], xt[b], pt[b], ALU.add), reads=[("x0", b), ("p0", b)], writes=[("x0", b)])
            P.dma("sp", r3(self.xa)[:, :, sl], xt[b], reads=[("x0", b)], writes=["xa"])
        P.dma("sp", pt[0][:, :, 0:CT], r3(self.ctxT), writes=[("p0", 0)])
        P.dma("sp", r3(self.ca), pt[0][:, :, 0:CT], reads=[("p0", 0)], writes=["ca"])
        self.apos = keep
        for blk in range(6):
            fw = 512 if blk < 5 else 256
            for j, w in enumerate((self.ffn_w1, self.ffn_w3)):
                P.dma("pool", self.f13b[blk, :, j, :, 0:fw],
                      w.rearrange("(k p) f -> p k f", p=128)[:, :, blk * 512:blk * 512 + fw], writes=["f13b"])
        for db in range(4):
            P.dma("pool", self.f2b[db], self.ffn_w2.rearrange("(fc p) c -> p fc c", p=128)[:, :, db * 256:(db + 1) * 256],
                  writes=["f2b"])
        for ex in range(NE):
            for blk in range(7):
                for j, w in enumerate((self.exp_w1, self.exp_w3)):
                    P.dma("pool", self.e13b[ex, blk, :, j],
                          w[ex].rearrange("(k p) f -> p k f", p=128)[:, :, blk * 512:(blk + 1) * 512], writes=[("e13b", ex)])
            for db in range(4):
                P.dma("pool", self.e2b[ex, db],
                      self.exp_w2[ex].rearrange("(fc p) c -> p fc c", p=128)[:, :, db * 256:(db + 1) * 256],
                      writes=[("e2b", ex)])

    def sweep_phase(self, l):
        P = self.P
        S, CT = self.S, self.CT
        last = (l == 1)
        xin, xout = (self.xa, self.xb)
        cin, cout = (self.ca, self.cb)
        r3 = lambda ap: ap.rearrange("(k p) t -> p k t", p=128)
        w_in16 = self.b16("w_in16", KC * PROJ).rearrange("p (k n) -> p k n", k=8)
        w_out16 = self.b16("w_out16", KC * D).rearrange("p (k n) -> p k n", k=8)
        P.dma("sp", w_in16, self.w_in_b[l], reads=[("w_in_b", l)], writes=["w_in16"])
        P.dma("sp", w_out16, self.w_out_b[l], reads=[("w_out_b", l)], writes=["w_out16"])
        cw = self.cs(f"convw{l}").rearrange("p (c j) -> p c j", j=5)
        nega = self.vec[(l, "nega")]
        dtb = self.cs(f"dtb{l}", rows=16)
        NB = 2
        A = lambda name, cols, n=NB: [self.f32(name, cols) for _ in range(n)]
        B = lambda name, cols, n=NB: [self.b16(name, cols) for _ in range(n)]
        xt = [a.rearrange("p (k n) -> p k n", k=8) for a in A("xt", KC * 132)]
        sq16 = [a.rearrange("p (k n) -> p k n", k=8) for a in B("sq16", KC * 132)]
        rinv = A("rinv", 132)
        tmp = [a.rearrange("p (k n) -> p k n", k=8) for a in A("tmp", KC * 132, 1)]
        h16 = [a.rearrange("p (k n) -> p k n", k=8) for a in B("h16", KC * 132)]
        pq = [a.rearrange("p (c n) -> p c n", c=12) for a in A("pq", 12 * 132, 1)]
        cy = [a.rearrange("p (c n) -> p c n", c=12) for a in A("cy", 12 * 128, 1)]
        sl32 = [a.rearrange("p (c n) -> p c n", c=12) for a in A("sl32", 12 * 128, 1)]
        sqk16 = B("sqk16", 1024, 1)
        rqk = A("rqk", 1024, 1)
        q16 = [a.rearrange("p (c n) -> p c n", c=4) for a in B("q16", 512)]
        k16 = [a.rearrange("p (c n) -> p c n", c=4) for a in B("k16", 512)]
        v16 = [a.rearrange("p (c n) -> p c n", c=4) for a in B("v16", 512)]
        gt = A("gt", 128 * 6, 1)
        tm = A("tm", 48)
        gcs = A("gcs", 8)
        sm = A("sm", 40)
        rbT = A("rbT", 128)
        zs = [a.rearrange("p (c n) -> p c n", c=4) for a in A("zs", 512, 1)]
        gu = [a.rearrange("p (c n) -> p c n", c=4) for a in A("gu", 512, 1)]
        gv = A("gv", 512, 1)
        gvn = A("gvn", 512, 1)
        gvn16 = B("gvn16", 512, 1)
        st6 = A("st6", 8, 1)
        o32 = [a.rearrange("p (c n) -> p c n", c=4) for a in A("o32", 512)]
        off = [a.rearrange("p (c n) -> p c n", c=4) for a in A("off", 512)]
        osq16 = B("osq16", 512, 1)
        orinv = A("orinv", 512, 1)
        ot = A("ot", 512, 1)
        cat16 = [a.rearrange("p (k n) -> p k n", k=8) for a in B("cat16", KC * 128)]
        mixt = A("mixt", 128, 1)
        xn = [a.rearrange("p (k n) -> p k n", k=8) for a in A("xn", KC * 128)]
        HB = 2
        Eq = A("Eq", 128, HB); Ea = A("Ea", 128, HB); EG = A("EG", 128, HB)
        M16 = B("M16", 128, HB); qkT16 = B("qkT16", 128, HB)
        Pb = [B("Pb", 128, HB) for _ in range(2)]
        Qb = [B("Qb", 128, HB) for _ in range(2)]
        Rb = [B("Rb", 128, HB) for _ in range(2)]
        kbg16 = B("kbg16", 128, HB); kdec16 = B("kdec16", 128, HB); vb16 = B("vb16", 128, HB)
        u32 = A("u32", 128, HB); wT16 = B("wT16", 128, HB); qd16 = B("qd16", 128, HB); vn16 = B("vn16", 128, HB)
        S32 = A("S32", 128, 4); S16 = B("S16", 128, 4)
        ong = self.cs(f"ong{l}")
        sgug = self.cs(f"sgug{l}")
        bsrow = self.cs(f"bsrow{l}").rearrange("p (g n) -> p g n", g=4)

        seqs = [("c", cin, cout, CT, 1, 0), ("x", xin, xout, S, 0, CT)]

        for dr in (0, 1):
            nm_incl = self.nm16["nm_f_incl" if dr == 0 else "nm_b_incl"]
            nm_strict = self.nm16["nm_f_strict" if dr == 0 else "nm_b_strict"]
            Lc = self.cs("Lf" if dr == 0 else "Lb")
            for h in range(4):
                P.op("dve", (lambda h: lambda e: e.memset(S32[h], 0.0))(h), writes=[("S32", h)])
                P.op("dve", (lambda h: lambda e: e.memset(S16[h], 0.0))(h), writes=[("S16", h)])
            tiles = []
            for sq in seqs:
                nt = sq[3] // 128
                order = range(nt) if dr == 0 else range(nt - 1, -1, -1)
                tiles += [(sq, t) for t in order]
            full = (dr == 1)

            def load(i):
                (name, xi, xo, L, strm, ofo), t = tiles[i]
                b = i % 2
                a0, a1 = max(0, t * 128 - 2), min(L, t * 128 + 130)
                c0 = a0 - (t * 128 - 2)
                if c0 > 0:
                    P.op("pool", (lambda b: lambda e: e.memset(xt[b][:, :, 0:2], 0.0))(b), writes=[("xt", b)])
                if a1 < t * 128 + 130:
                    P.op("pool", (lambda b: lambda e: e.memset(xt[b][:, :, 130:132], 0.0))(b), writes=[("xt", b)])
                P.dma("sp", xt[b][:, :, c0:c0 + (a1 - a0)], r3(xi)[:, :, a0:a1], reads=[("xsrc", name)], writes=[("xt", b)])
                if full:
                    P.dma("sp", off[b], self.of.rearrange("(h p) t -> p h t", p=128)[:, :, ofo + t * 128: ofo + (t + 1) * 128],
                          reads=[("of", name, t)], writes=[("off", b)])

            load(0)
            for i in range(len(tiles)):
                if i + 1 < len(tiles):
                    load(i + 1)
                (name, xi, xo, L, strm, ofo), t = tiles[i]
                b = i % 2
                V = self.vec[(l, strm)]
                do_out = full and not (last and name == "c")
                P.op("act", act(sq16[b], xt[b], AF.Square), reads=[("xt", b)], writes=[("sq16", b)])
                ps, pk = self.ps_half()
                for kc in range(KC):
                    P.op("pe", mm(ps[:, 0:132], self.ones16, sq16[b][:, kc, :], kc == 0, kc == KC - 1),
                         reads=[("sq16", b), "ones16"], writes=[pk])
                P.op("act", act(rinv[b], ps[:, 0:132], AF.Ln, bias=EPS, scale=1.0 / D), reads=[pk], writes=[("rinv", b)])
                P.op("act", act(rinv[b], rinv[b], AF.Exp, scale=-0.5), reads=[("rinv", b)], writes=[("rinv", b)])
                for kc in range(KC):
                    P.op("dve", (lambda kc, b, V: lambda e: e.scalar_tensor_tensor(
                        out=tmp[0][:, kc, :], in0=xt[b][:, kc, :], scalar=V["gs1"][:, kc:kc + 1], in1=rinv[b],
                        op0=ALU.mult, op1=ALU.mult))(kc, b, V),
                        reads=[("xt", b), ("rinv", b), ("vec", l, strm)], writes=[("tmp", kc)])
                    P.op("act", act(h16[b][:, kc, :], tmp[0][:, kc, :], AF.Identity, bias=V["sh1"][:, kc:kc + 1], scale=1.0),
                         reads=[("tmp", kc), ("vec", l, strm)], writes=[("h16", b)])
                ocs = list(range(12)) + [16]
                if do_out:
                    ocs += [12, 13, 14, 15, 17, 18, 19, 20]
                for oc in ocs:
                    n0 = {16: 2048}.get(oc, oc * 128 if oc < 16 else 2064 + (oc - 17) * 128)
                    rows = 16 if oc == 16 else 128
                    ps, pk = self.ps_half()
                    for kc in range(KC):
                        P.op("pe", mm(ps[0:rows, 0:132], w_in16[:, kc, n0:n0 + rows], h16[b][:, kc, :], kc == 0, kc == KC - 1),
                             reads=["w_in16", ("h16", b)], writes=[pk])
                    if oc < 12:
                        P.op("act", act(pq[0][:, oc, :], ps[:, 0:132], AF.Copy), reads=[pk], writes=[("pq", oc)])
                    elif oc < 16:
                        P.op("act", act(zs[0][:, oc - 12, :], ps[:, 2:130], AF.Silu), reads=[pk], writes=[("zs", oc - 12)])
                    elif oc == 16:
                        g = gt[0].rearrange("p (r n) -> p r n", r=6)
                        P.op("dve", ts(g[0:16, 0, :], ps[0:16, 2:130], dtb, ALU.add), reads=[pk, "cst"], writes=["gt0"])
                        P.op("act", act(g[0:16, 4, :], ps[0:16, 2:130], AF.Sigmoid), reads=[pk], writes=["gt4"])
                    else:
                        P.op("act", act(gu[0][:, oc - 17, :], ps[:, 2:130], AF.Gelu_apprx_tanh), reads=[pk], writes=[("gu", oc - 17)])
                if do_out:
                    ps, pk = self.ps_full(0, 3)
                    for kc in range(KC):
                        P.op("pe", mm(ps, h16[b][:, kc, 2:130], w_in16[:, kc, 2576:3088], kc == 0, kc == KC - 1),
                             reads=["w_in16", ("h16", b)], writes=[pk])
                    P.op("act", act(gv[0], ps, AF.Gelu_apprx_tanh), reads=[pk], writes=["gv"])
                pqk = [("pq", oc) for oc in range(12)]
                if t == 0:
                    P.op("pool", ms(pq[0][:, :, 0:2], 0.0), reads=pqk, writes=pqk)
                if t == L // 128 - 1:
                    P.op("pool", ms(pq[0][:, :, 130:132], 0.0), reads=pqk, writes=pqk)
                for oc in range(12):
                    P.op("act", act(cy[0][:, oc, :], pq[0][:, oc, 0:128], AF.Copy, scale=cw[:, oc, 0:1]),
                         reads=[("pq", oc), "cst"], writes=[("cy", oc)])
                    for j in range(1, 5):
                        P.op("dve", (lambda oc, j: lambda e: e.scalar_tensor_tensor(
                            out=cy[0][:, oc, :], in0=pq[0][:, oc, j:j + 128], scalar=cw[:, oc, j:j + 1], in1=cy[0][:, oc, :],
                            op0=ALU.mult, op1=ALU.add))(oc, j),
                            reads=[("pq", oc), ("cy", oc), "cst"], writes=[("cy", oc)])
                cyk = [("cy", oc) for oc in range(12)]
                P.op("act", act(sl32[0], cy[0], AF.Silu), reads=cyk, writes=["sl32"])
                sl_flat = sl32[0].rearrange("p c n -> p (c n)")
                P.op("act", act(sqk16[0], sl_flat[:, 0:1024], AF.Square), reads=["sl32"], writes=["sqk16"])
                for hf in range(2):
                    ps, pk = self.ps_full(0, 3)
                    P.op("pe", mm(ps, self.ones16, sqk16[0][:, hf * 512:(hf + 1) * 512]), reads=["sqk16", "ones16"],
                         writes=[pk])
                    P.op("act", act(rqk[0][:, hf * 512:(hf + 1) * 512], ps, AF.Ln, bias=EPS, scale=1.0),
                         reads=[pk], writes=[("rqk", hf)])
                    P.op("act", act(rqk[0][:, hf * 512:(hf + 1) * 512], rqk[0][:, hf * 512:(hf + 1) * 512], AF.Exp, scale=-0.5),
                         reads=[("rqk", hf)], writes=[("rqk", hf)])
                P.op("dve", (lambda b: lambda e: e.scalar_tensor_tensor(
                    out=q16[b].rearrange("p c n -> p (c n)"), in0=sl_flat[:, 0:512], scalar=128.0 ** -0.5, in1=rqk[0][:, 0:512],
                    op0=ALU.mult, op1=ALU.mult))(b), reads=["sl32", ("rqk", 0)], writes=[("q16", b)])
                P.op("dve", (lambda b: lambda e: e.tensor_tensor(
                    out=k16[b].rearrange("p c n -> p (c n)"), in0=sl_flat[:, 512:1024], in1=rqk[0][:, 512:1024], op=ALU.mult))(b),
                    reads=["sl32", ("rqk", 1)], writes=[("k16", b)])
                P.op("pool", (lambda b: lambda e: e.tensor_copy(out=v16[b].rearrange("p c n -> p (c n)"), in_=sl_flat[:, 1024:1536]))(b),
                     reads=["sl32"], writes=[("v16", b)])
                g = gt[0].rearrange("p (r n) -> p r n", r=6)
                R16 = lambda r: g[0:16, r, :]
                P.op("act", act(R16(1), R16(0), AF.Abs), reads=["gt0"], writes=["gt1"])
                P.op("act", act(R16(1), R16(1), AF.Exp, scale=-1.0), reads=["gt1"], writes=["gt1"])
                P.op("act", act(R16(1), R16(1), AF.Ln, bias=1.0, scale=1.0), reads=["gt1"], writes=["gt1"])
                P.op("dve", stt(R16(2), R16(0), 0.0, R16(1), ALU.max, ALU.add), reads=["gt0", "gt1"], writes=["gt2"])
                P.op("dve", ts(R16(3), R16(2), nega[0:16, :], ALU.mult), reads=["gt2", ("nega", l)], writes=["gt3"])
                P.op("dve", ts(R16(5), R16(4), 1e-30, ALU.max), reads=["gt4"], writes=["gt5"])
                P.op("act", act(R16(5), R16(5), AF.Ln), reads=["gt5"], writes=["gt5"])
                psq, pkq = self.ps_q()
                for j, r in enumerate((3, 4, 5)):
                    P.op("pe", tr(psq[:, j * 16:(j + 1) * 16], R16(r), self.cs("ident", 0, 16, rows=16)),
                         reads=[f"gt{r}", "cst"], writes=[pkq])
                P.op("act", act(tm[b], psq[:, 0:48], AF.Copy), reads=[pkq], writes=[("tm", b)])
                gcol = tm[b][:, dr * 8: dr * 8 + 4]
                bcol = tm[b][:, 16 + dr * 8 + 4: 16 + dr * 8 + 8]
                lcol = tm[b][:, 32 + dr * 8 + 4: 32 + dr * 8 + 8]
                psq, pkq = self.ps_q()
                P.op("pe", mm(psq[:, 0:4], Lc, gcol), reads=[("tm", b), "cst"], writes=[pkq])
                P.op("pe", mm(psq[:, 4:8], self.cs("ones"), gcol), reads=[("tm", b), "cst"], writes=[pkq])
                P.op("act", act(gcs[b], psq[:, 0:8], AF.Copy), reads=[pkq], writes=[("gcs", b)])
                smb = sm[b]
                gc, tot = gcs[b][:, 0:4], gcs[b][:, 4:8]
                ngc, dd, kd, eg, bge, glast, rbs = (smb[:, 4:8], smb[:, 8:12], smb[:, 12:16], smb[:, 16:20],
                                                    smb[:, 20:24], smb[:, 24:28], smb[:, 28:36])
                smk = ("sm", b)
                P.op("dve", cp(rbs[:, 0:4], gc), reads=[("gcs", b)], writes=[smk])
                P.op("dve", tt(rbs[:, 4:8], gc, lcol, ALU.add), reads=[("gcs", b), ("tm", b)], writes=[smk])
                P.op("dve", ts(ngc, gc, -1.0, ALU.mult), reads=[("gcs", b)], writes=[smk])
                P.op("dve", tt(dd, tot, gc, ALU.subtract), reads=[("gcs", b)], writes=[smk])
                P.op("act", act(kd, dd, AF.Exp), reads=[smk], writes=[smk])
                P.op("act", act(eg, gc, AF.Exp), reads=[("gcs", b)], writes=[smk])
                P.op("act", act(glast, tot, AF.Exp), reads=[("gcs", b)], writes=[smk])
                P.op("dve", tt(bge, eg, bcol, ALU.mult), reads=[smk, ("tm", b)], writes=[smk])
                pst, pkt = self.ps_q()
                P.op("pe", tr(pst[0:8, 0:128], rbs, self.cs("ident")), reads=[smk, "cst"], writes=[pkt])
                P.op("act", act(rbT[b][0:8, :], pst[0:8, 0:128], AF.Copy), reads=[pkt], writes=[("rbT", b)])
                sel = self.cs("sel", rows=8).rearrange("p (r m) -> p r m", r=8)
                for h in range(4):
                    hb = h % HB
                    K = lambda nme: (nme, hb)
                    pGq, kGq = self.ps_q()
                    P.op("pe", mm(pGq, sel[:, h, :], rbT[b][0:8, :], True, False), reads=[("rbT", b), "cst"], writes=[kGq])
                    P.op("pe", mm(pGq, self.ident16, nm_incl, False, True), reads=["ident16", "nm_f_incl", "nm_b_incl"], writes=[kGq])
                    pGa, kGa = self.ps_q()
                    P.op("pe", mm(pGa, sel[:, 4 + h, :], rbT[b][0:8, :], True, False), reads=[("rbT", b), "cst"], writes=[kGa])
                    P.op("pe", mm(pGa, self.ident16, nm_strict, False, True), reads=["ident16", "nm_f_strict", "nm_b_strict"], writes=[kGa])
                    pGe, kGe = self.ps_q()
                    P.op("pe", mm(pGe, sel[:, h, :], rbT[b][0:8, :]), reads=[("rbT", b), "cst"], writes=[kGe])
                    P.op("act", act(Eq[hb], pGq, AF.Exp, bias=ngc[:, h:h + 1], scale=1.0), reads=[kGq, smk], writes=[K("Eq")])
                    P.op("act", act(Ea[hb], pGa, AF.Exp, bias=ngc[:, h:h + 1], scale=1.0), reads=[kGa, smk], writes=[K("Ea")])
                    P.op("act", act(EG[hb], pGe, AF.Exp), reads=[kGe], writes=[K("EG")])
                    pKK, kKK = self.ps_q()
                    P.op("pe", mm(pKK, k16[b][:, h, :], k16[b][:, h, :]), reads=[("k16", b)], writes=[kKK])
                    pQK, kQK = self.ps_q()
                    P.op("pe", mm(pQK, k16[b][:, h, :], q16[b][:, h, :]), reads=[("k16", b), ("q16", b)], writes=[kQK])
                    P.op("dve", (lambda hb, pKK: lambda e: e.scalar_tensor_tensor(
                        out=M16[hb], in0=pKK, scalar=-1.0, in1=Ea[hb], op0=ALU.mult, op1=ALU.mult))(hb, pKK),
                        reads=[kKK, K("Ea")], writes=[K("M16")])
                    P.op("dve", (lambda hb, pQK: lambda e: e.tensor_tensor(out=qkT16[hb], in0=pQK, in1=Eq[hb], op=ALU.mult))(hb, pQK),
                         reads=[kQK, K("Eq")], writes=[K("qkT16")])
                    pN, kN = self.ps_t()
                    P.op("pe", (lambda pN, hb: lambda e: e.transpose(out=pN, in_=M16[hb], identity=self.ident16))(pN, hb),
                         reads=[K("M16"), "ident16"], writes=[kN])
                    Nf, Nfk = Qb[0][hb], ("Qb", 0, hb)
                    P.op("act", act(Nf, pN, AF.Copy), reads=[kN], writes=[Nfk])
                    P.op("pool", tt(Pb[0][hb], M16[hb], self.nm16["D2"], ALU.mult), reads=[K("M16"), "D2"], writes=[("Pb", 0, hb)])
                    P.op("pool", tt(Rb[0][hb], Pb[0][hb], self.ident16, ALU.add), reads=[("Pb", 0, hb), "ident16"], writes=[("Rb", 0, hb)])
                    for li, sz in enumerate((2, 4, 8, 16, 32, 64)):
                        src, dst = li % 2, (li + 1) % 2
                        Xc, Xck = Rb[src][hb], ("Rb", src, hb)
                        Ns, Nsk = Qb[1][hb], ("Qb", 1, hb)
                        P.op("pool", tt(Ns, Nf, self.nm16[f"B{sz}"], ALU.mult), reads=[Nfk, f"B{sz}"], writes=[Nsk])
                        pY, kY = self.ps_q()
                        P.op("pe", mm(pY, Ns, Xc), reads=[Nsk, Xck], writes=[kY])
                        Y16, Yk = Pb[1][hb], ("Pb", 1, hb)
                        P.op("dve", cp(Y16, pY), reads=[kY], writes=[Yk])
                        pXT, kXT = self.ps_t()
                        P.op("pe", tr(pXT, Xc, self.ident16), reads=[Xck, "ident16"], writes=[kXT])
                        XT16, XTk = Pb[0][hb], ("Pb", 0, hb)
                        P.op("act", act(XT16, pXT, AF.Copy), reads=[kXT], writes=[XTk])
                        pR, kR = self.ps_q()
                        P.op("pe", mm(pR, XT16, Y16, True, False), reads=[XTk, Yk], writes=[kR])
                        P.op("pe", mm(pR, self.ident16, Xc, False, True), reads=["ident16", Xck], writes=[kR])
                        if li % 2:
                            P.op("dve", cp(Rb[dst][hb], pR), reads=[kR], writes=[("Rb", dst, hb)])
                        else:
                            P.op("act", act(Rb[dst][hb], pR, AF.Copy), reads=[kR], writes=[("Rb", dst, hb)])
                    RT, RTk = Rb[0][hb], ("Rb", 0, hb)
                    pkt_, kkt = self.ps_t()
                    P.op("pe", (lambda p_, b, h: lambda e: e.transpose(out=p_, in_=k16[b][:, h, :], identity=self.ident16))(pkt_, b, h),
                         reads=[("k16", b), "ident16"], writes=[kkt])
                    pvt, kvt = self.ps_t()
                    P.op("pe", (lambda p_, b, h: lambda e: e.transpose(out=p_, in_=v16[b][:, h, :], identity=self.ident16))(pvt, b, h),
                         reads=[("v16", b), "ident16"], writes=[kvt])
                    P.op("act", act(kbg16[hb], pkt_, AF.Copy, scale=bge[:, h:h + 1]), reads=[kkt, smk], writes=[K("kbg16")])
                    P.op("act", act(kdec16[hb], pkt_, AF.Copy, scale=kd[:, h:h + 1]), reads=[kkt, smk], writes=[K("kdec16")])
                    P.op("dve", (lambda hb, pvt, h, bcol: lambda e: e.tensor_scalar(
                        out=vb16[hb], in0=pvt, scalar1=bcol[:, h:h + 1], scalar2=None, op0=ALU.mult))(hb, pvt, h, bcol),
                        reads=[kvt, ("tm", b)], writes=[K("vb16")])
                    pu, ku = self.ps_q()
                    P.op("pe", mm(pu, RT, vb16[hb]), reads=[RTk, K("vb16")], writes=[ku])
                    pw, kw = self.ps_q()
                    P.op("pe", mm(pw, kbg16[hb], RT), reads=[RTk, K("kbg16")], writes=[kw])
                    P.op("act", act(u32[hb], pu, AF.Copy), reads=[ku], writes=[K("u32")])
                    P.op("dve", (lambda hb, pw: lambda e: e.tensor_copy(out=wT16[hb], in_=pw))(hb, pw), reads=[kw], writes=[K("wT16")])
                    P.op("pool", (lambda hb, b, h: lambda e: e.tensor_tensor(out=qd16[hb], in0=q16[b][:, h, :], in1=EG[hb], op=ALU.mult))(hb, b, h),
                         reads=[("q16", b), K("EG")], writes=[K("qd16")])
                    pws, kws = self.ps_q()
                    P.op("pe", mm(pws, wT16[hb], S16[h]), reads=[K("wT16"), ("S16", h)], writes=[kws])
                    P.op("dve", (lambda hb, pws: lambda e: e.tensor_tensor(out=vn16[hb], in0=u32[hb], in1=pws, op=ALU.subtract))(hb, pws),
                         reads=[kws, K("u32")], writes=[K("vn16")])
                    po, ko = self.ps_q()
                    P.op("pe", mm(po, S16[h], qd16[hb], True, False), reads=[("S16", h), K("qd16")], writes=[ko])
                    P.op("pe", mm(po, vn16[hb], qkT16[hb], False, True), reads=[K("vn16"), K("qkT16")], writes=[ko])
                    psn, ksn = self.ps_q()
                    P.op("pe", mm(psn, kdec16[hb], vn16[hb]), reads=[K("kdec16"), K("vn16")], writes=[ksn])
                    P.op("dve", (lambda h, psn, glast: lambda e: e.scalar_tensor_tensor(
                        out=S32[h], in0=S32[h], scalar=glast[:, h:h + 1], in1=psn, op0=ALU.mult, op1=ALU.add))(h, psn, glast),
                        reads=[ksn, smk, ("S32", h)], writes=[("S32", h)])
                    P.op("act", act(S16[h], S32[h], AF.Copy), reads=[("S32", h)], writes=[("S16", h)])
                    if not full:
                        P.op("act", act(o32[b][:, h, :], po, AF.Copy), reads=[ko], writes=[("o32", b)])
                    else:
                        P.op("dve", (lambda b, h, po: lambda e: e.tensor_tensor(out=o32[b][:, h, :], in0=po, in1=off[b][:, h, :], op=ALU.add))(b, h, po),
                             reads=[ko, ("off", b)], writes=[("o32", b)])
                if not full:
                    P.dma("sp", self.of.rearrange("(h p) t -> p h t", p=128)[:, :, ofo + t * 128: ofo + (t + 1) * 128], o32[b],
                          reads=[("o32", b)], writes=[("of", name, t)])
                    continue
                if not do_out:
                    continue
                o_flat = o32[b].rearrange("p c n -> p (c n)")
                P.op("act", act(osq16[0], o_flat, AF.Square), reads=[("o32", b)], writes=["osq16"])
                ps, pk = self.ps_full(0, 3)
                pks = [pk]
                P.op("pe", mm(ps, self.ones16, osq16[0]), reads=["osq16", "ones16"], writes=pks)
                P.op("act", act(orinv[0], ps, AF.Ln, bias=EPS, scale=1.0 / 128.0), reads=pks, writes=["orinv"])
                P.op("act", act(orinv[0], orinv[0], AF.Exp, scale=-0.5), reads=["orinv"], writes=["orinv"])
                P.op("dve", stt(ot[0], o_flat, ong, orinv[0], ALU.mult, ALU.mult), reads=[("o32", b), "orinv", "cst"], writes=["ot"])
                zk = [("zs", c) for c in range(4)]
                P.op("pool", (lambda b: lambda e: e.tensor_tensor(
                    out=cat16[b][:, 0:4, :].rearrange("p c n -> p (c n)"), in0=ot[0], in1=zs[0].rearrange("p c n -> p (c n)"), op=ALU.mult))(b),
                    reads=["ot"] + zk, writes=[("cat16", b)])
                P.op("dve", (lambda: lambda e: e.bn_stats(out=st6[0][:, 0:6], in_=gv[0]))(), reads=["gv"], writes=["st6"])
                P.op("dve", (lambda: lambda e: e.bn_aggr(out=st6[0][:, 6:8], in_=st6[0][:, 0:6]))(), reads=["st6"], writes=["st6"])
                P.op("act", act(st6[0][:, 7:8], st6[0][:, 7:8], AF.Ln, bias=EPS, scale=1.0), reads=["st6"], writes=["st6"])
                P.op("act", act(st6[0][:, 7:8], st6[0][:, 7:8], AF.Exp, scale=-0.5), reads=["st6"], writes=["st6"])
                P.op("dve", ts(gvn[0], gv[0], st6[0][:, 6:7], ALU.subtract, st6[0][:, 7:8], ALU.mult), reads=["gv", "st6"], writes=["gvn"])
                P.op("pool", tt(gvn16[0], gvn[0], sgug, ALU.mult), reads=["gvn", "cst"], writes=["gvn16"])
                for gi in range(4):
                    pm, km = self.ps_q()
                    P.op("pe", mm(pm, gvn16[0][:, gi * 128:(gi + 1) * 128], self.wsT16[:, l * 512 + gi * 128: l * 512 + (gi + 1) * 128]),
                         reads=["gvn16", "wsT16"], writes=[km])
                    P.op("dve", (lambda pm, gi: lambda e: e.tensor_tensor(out=mixt[0], in0=pm, in1=bsrow[:, gi, :], op=ALU.add))(pm, gi),
                         reads=[km, "cst"], writes=["mixt"])
                    P.op("pool", (lambda b, gi: lambda e: e.tensor_tensor(out=cat16[b][:, 4 + gi, :], in0=mixt[0], in1=gu[0][:, gi, :], op=ALU.mult))(b, gi),
                         reads=["mixt", ("gu", gi)], writes=[("cat16", b)])
                for dc in range(KC):
                    py, ky = self.ps_q()
                    for kc in range(KC):
                        P.op("pe", mm(py, w_out16[:, kc, dc * 128:(dc + 1) * 128], cat16[b][:, kc, :], kc == 0, kc == KC - 1),
                             reads=["w_out16", ("cat16", b)], writes=[ky])
                    P.op("dve", (lambda b, dc, py, V: lambda e: e.scalar_tensor_tensor(
                        out=xn[b][:, dc, :], in0=py, scalar=V["g1"][:, dc:dc + 1], in1=xt[b][:, dc, 2:130],
                        op0=ALU.mult, op1=ALU.add))(b, dc, py, V),
                        reads=[ky, ("xt", b), ("vec", l, strm)], writes=[("xn", b)])
                P.dma("sp", r3(xo)[:, :, t * 128:(t + 1) * 128], xn[b], reads=[("xn", b)], writes=[("xdst", name)])

    def mlp_phase(self, l):
        P = self.P
        S, CT, NT = self.S, self.CT, self.NT
        last = (l == 1)
        moe = last
        r3 = lambda ap: ap.rearrange("(k p) t -> p k t", p=128)
        xtile = self.f32("xtile", KC * NT).rearrange("p (k n) -> p k n", k=8)
        acc = self.f32("acc", KC * NT).rearrange("p (k n) -> p k n", k=8)
        h16 = self.b16("h16m", KC * NT).rearrange("p (k n) -> p k n", k=8)
        rinv = self.f32("rinvm", NT)
        NHID = 12
        hid = self.b16("hid", NHID * NT).rearrange("p (c n) -> p c n", c=NHID)
        sq16 = hid[:, 0:KC, :]
        sqk = [("hid", c) for c in range(KC)]
        sil = [self.f32("sil", NT) for _ in range(2)]
        ytmp = sil[1]
        NWB = 2
        w13 = [self.b16("w13", 2 * KC * 512).rearrange("p (j k f) -> p j k f", j=2, k=8) for _ in range(NWB)]
        w2 = [self.b16("w2", NHID * 256).rearrange("p (c n) -> p c n", c=NHID) for _ in range(2)]
        gbc = self.f32("gbc", NT)
        gT = self.f32("gT", NT)
        rs = self.f32("rs", 64)
        sel = self.cs("sel", rows=8).rearrange("p (r m) -> p r m", r=8)
        T = ALU
        if moe:
            experts = [(self.e13b[ex], self.e2b[ex], 28, [(0, 3), (3, 2), (5, 2)], ("e13b", ex), ("e2b", ex), ex) for ex in range(NE)]
        else:
            experts = [(self.f13b, self.f2b, 22, [(0, 3), (3, 3)], "f13b", "f2b", None)]
        streams = [("x", self.xb, self.outT if last else self.xa, S, 0)]
        if not last:
            streams.append(("c", self.cb, self.ca, CT, 1))
        aks = [("accd", dc) for dc in range(KC)]
        nw13 = 0
        nw2 = 0
        for (name, xi, xo, L, strm) in streams:
            V = self.vec[(l, strm)]
            n = min(NT, L)
            NS = [(s0, min(512, n - s0)) for s0 in range(0, n, 512)]

            def rms_rinv(scale):
                P.op("act", act(sq16[:, :, 0:n], xtile[:, :, 0:n], AF.Square), reads=["xtile"], writes=sqk)
                for (s0, sn) in NS:
                    ps, pk = self.ps_full(0, 3)
                    for kc in range(KC):
                        P.op("pe", mm(ps[:, 0:sn], self.ones16, sq16[:, kc, s0:s0 + sn], kc == 0, kc == KC - 1),
                             reads=sqk + ["ones16"], writes=[pk])
                    P.op("act", act(rinv[:, s0:s0 + sn], ps[:, 0:sn], AF.Ln, bias=EPS, scale=scale), reads=[pk], writes=["rinvm"])
                P.op("act", act(rinv[:, 0:n], rinv[:, 0:n], AF.Exp, scale=-0.5), reads=["rinvm"], writes=["rinvm"])

            for t in range(L // n):
                tsl = slice(t * n, (t + 1) * n)
                P.dma("sp", xtile[:, :, 0:n], r3(xi)[:, :, tsl], reads=[("xdst", name)], writes=["xtile"])
                rms_rinv(1.0 / D)
                for kc in range(KC):
                    P.op("dve", stt(acc[:, kc, 0:n], xtile[:, kc, 0:n], V["gs2"][:, kc:kc + 1], rinv[:, 0:n], T.mult, T.mult),
                         reads=["xtile", "rinvm", ("vec", l, strm)], writes=["acc"] + aks)
                    P.op("act", act(acc[:, kc, 0:n], acc[:, kc, 0:n], AF.Identity, bias=V["sh2"][:, kc:kc + 1], scale=1.0),
                         reads=["acc", ("vec", l, strm)], writes=["acc"])
                P.op("pool", cp(h16[:, :, 0:n], acc[:, :, 0:n]), reads=["acc"], writes=["h16m"])
                if moe:
                    lg, m1, mk1, l2, m2, mk2, em, ssum = (rs[:, 0:8], rs[:, 8:9], rs[:, 16:24], rs[:, 24:32], rs[:, 9:10],
                                                          rs[:, 32:40], rs[:, 40:48], rs[:, 10:11])
                    for st_ in range(n // 128):
                        pr, kr = self.ps_q()
                        for kc in range(KC):
                            P.op("pe", mm(pr[:, 0:8], acc[:, kc, st_ * 128:(st_ + 1) * 128], self.rw32[:, kc * 8:(kc + 1) * 8],
                                          kc == 0, kc == KC - 1), reads=["acc", "rw32"], writes=[kr])
                        R = dict(reads=["rs"], writes=["rs"])
                        P.op("dve", tt(lg, pr[:, 0:8], self.cs("rbrow"), T.add), reads=[kr, "cst", "rs"], writes=["rs"])
                        P.op("dve", red(m1, lg, T.max), **R)
                        P.op("dve", ts(mk1, lg, m1, T.is_equal), **R)
                        P.op("dve", stt(l2, mk1, -1e30, lg, T.mult, T.add), **R)
                        P.op("dve", red(m2, l2, T.max), **R)
                        P.op("dve", ts(mk2, l2, m2, T.is_equal), **R)
                        P.op("dve", tt(mk1, mk1, mk2, T.add), **R)
                        P.op("dve", ts(l2, lg, m1, T.subtract), **R)
                        P.op("act", act(l2, l2, AF.Exp), **R)
                        P.op("dve", tt(em, l2, mk1, T.mult), **R)
                        P.op("dve", red(ssum, em, T.add), **R)
                        P.op("dve", (lambda: lambda e: e.reciprocal(out=ssum, in_=ssum))(), **R)
                        P.op("dve", ts(em, em, ssum, T.mult), **R)
                        pt_, kt_ = self.ps_q()
                        P.op("pe", tr(pt_[0:8, 0:128], em, self.cs("ident")), reads=["rs", "cst"], writes=[kt_])
                        P.op("act", act(gT[0:8, st_ * 128:(st_ + 1) * 128], pt_[0:8, 0:128], AF.Copy), reads=[kt_], writes=["gT"])
                blocks = []
                for ei, (e13, e2, nfc, groups, k13, k2, ex) in enumerate(experts):
                    for gi, (b0, nb) in enumerate(groups):
                        for bi in range(b0, b0 + nb):
                            blocks.append((ei, gi, bi, bi == b0, bi == b0 + nb - 1))

                def load13(i):
                    ei, gi, bi, _, _ = blocks[i]
                    e13, k13, nfc_ = experts[ei][0], experts[ei][4], experts[ei][2]
                    fw = min(4, nfc_ - bi * 4) * 128
                    P.dma("sp", w13[(nw13 + i) % NWB][:, :, :, 0:fw], e13[bi][:, :, :, 0:fw], reads=[k13],
                          writes=[("w13", (nw13 + i) % NWB)])

                load13(0)
                first = True
                fcs = []
                for i, (ei, gi, bi, gfirst, glast_) in enumerate(blocks):
                    (e13, e2, nfc, groups, k13, k2, ex) = experts[ei]
                    if i + 1 < len(blocks):
                        load13(i + 1)
                    wb = (nw13 + i) % NWB
                    if gfirst:
                        fcs = []
                        if ex is not None and gi == 0:
                            for (s0, sn) in NS:
                                pg, kg = self.ps_full(0, 3)
                                P.op("pe", mm(pg[:, 0:sn], sel[:, ex, :], gT[0:8, s0:s0 + sn]), reads=["gT", "cst"], writes=[kg])
                                P.op("act", act(gbc[:, s0:s0 + sn], pg[:, 0:sn], AF.Copy), reads=[kg], writes=["gbc"])
                    nf = min(4, nfc - bi * 4)
                    for j in range(nf):
                        ci = len(fcs)
                        fcs.append(bi * 4 + j)
                        for (s0, sn) in NS:
                            pa, ka = self.ps_full(3, 4)
                            pb_, kb_ = self.ps_full(3, 4)
                            for kc in range(KC):
                                P.op("pe", mm(pa[:, 0:sn], w13[wb][:, 0, kc, j * 128:(j + 1) * 128], h16[:, kc, s0:s0 + sn],
                                              kc == 0, kc == KC - 1), reads=[("w13", wb), "h16m"], writes=[ka])
                            for kc in range(KC):
                                P.op("pe", mm(pb_[:, 0:sn], w13[wb][:, 1, kc, j * 128:(j + 1) * 128], h16[:, kc, s0:s0 + sn],
                                              kc == 0, kc == KC - 1), reads=[("w13", wb), "h16m"], writes=[kb_])
                            P.op("act", act(sil[0][:, s0:s0 + sn], pa[:, 0:sn], AF.Silu), reads=[ka], writes=[("sil", 0, s0)])
                            P.op("dve", tt(hid[:, ci, s0:s0 + sn], pb_[:, 0:sn], sil[0][:, s0:s0 + sn], T.mult),
                                 reads=[kb_, ("sil", 0, s0)], writes=[("hid", ci)])
                    if not glast_:
                        continue
                    ng = len(fcs)
                    for db in range(4):
                        w2b_ = nw2 % 2
                        nw2 += 1
                        P.dma("sp", w2[w2b_][:, 0:ng, :], e2[db][:, fcs[0]:fcs[0] + ng, :], reads=[k2], writes=[("w2", w2b_)])
                        for dj in range(2):
                            dc = db * 2 + dj
                            ak = ("accd", dc)
                            for (s0, sn) in NS:
                                py, ky = self.ps_full(3, 4)
                                for ci in range(ng):
                                    P.op("pe", mm(py[:, 0:sn], w2[w2b_][:, ci, dj * 128:(dj + 1) * 128], hid[:, ci, s0:s0 + sn],
                                                  ci == 0, ci == ng - 1), reads=[("w2", w2b_), ("hid", ci)], writes=[ky])
                                o_ = acc[:, dc, s0:s0 + sn]
                                if first:
                                    if ex is None:
                                        P.op("act", act(o_, py[:, 0:sn], AF.Copy), reads=[ky], writes=[ak, "acc"])
                                    else:
                                        P.op("dve", tt(o_, py[:, 0:sn], gbc[:, s0:s0 + sn], T.mult), reads=[ky, "gbc"], writes=[ak, "acc"])
                                elif ex is None:
                                    P.op("dve", tt(o_, py[:, 0:sn], o_, T.add), reads=[ky, ak], writes=[ak])
                                else:
                                    P.op("dve", tt(ytmp[:, s0:s0 + sn], py[:, 0:sn], gbc[:, s0:s0 + sn], T.mult),
                                         reads=[ky, "gbc"], writes=[("sil", 1, s0)])
                                    P.op("pool", tt(o_, o_, ytmp[:, s0:s0 + sn], T.add), reads=[("sil", 1, s0), ak], writes=[ak])
                    first = False
                nw13 += len(blocks)
                for dc in range(KC):
                    P.op("dve", stt(xtile[:, dc, 0:n], acc[:, dc, 0:n], V["g2"][:, dc:dc + 1], xtile[:, dc, 0:n], T.mult, T.add),
                         reads=[("accd", dc), "xtile", ("vec", l, strm)], writes=["xtile"])
                if last:
                    rms_rinv(1.0 / D)
                    fg = self.cs("fing")
                    for kc in range(KC):
                        P.op("dve", stt(xtile[:, kc, 0:n], xtile[:, kc, 0:n], fg[:, kc:kc + 1], rinv[:, 0:n], T.mult, T.mult),
                             reads=["xtile", "rinvm", "cst"], writes=["xtile"])
                tok = P.dma("sp", r3(xo)[:, :, tsl], xtile[:, :, 0:n], reads=["xtile"], writes=[("xsrc", name)])
                if last:
                    self.out_toks.append(tok)


_NC_CACHE = {}


def _get_nc(S, CT, debug=False):
    key = (S, CT, debug)
    if key not in _NC_CACHE:
        bld = Builder(S, CT, debug)
        _NC_CACHE[key] = bld.build()
        _NC_CACHE[("bld",) + key] = bld
    return _NC_CACHE[key]


def make_in_maps(inp, nb):
    S = inp["x"].shape[1]
    A = lambda v: np.ascontiguousarray(np.asarray(v, np.float32))
    posT = grid_pos_embed_T(S, D)
    wsT = A(np.transpose(np.asarray(inp["w_s"], np.float32), (0, 3, 1, 2)).reshape(2, 128, 512))
    shared = {
        "posT": posT, "wsT": wsT,
        "ffn_w1": A(inp["ffn_w1"][0]), "ffn_w3": A(inp["ffn_w3"][0]), "ffn_w2": A(inp["ffn_w2"][0]),
        "exp_w1": A(inp["exp_w1"][0]), "exp_w3": A(inp["exp_w3"][0]), "exp_w2": A(inp["exp_w2"][0]),
        "router_w": A(inp["router_w"][0]),
    }
    for l in range(2):
        shared[f"w_mod{l}"] = A(inp["w_mod"][l])
        shared[f"w_in{l}"] = A(inp["w_in"][l])
        shared[f"w_out{l}"] = A(inp["w_out"][l])
    maps = []
    for b in range(nb):
        m = dict(shared)
        m["xT"] = A(np.asarray(inp["x"][b], np.float32).T)
        m["ctxT"] = A(np.asarray(inp["ctx"][b], np.float32).T)
        m["cst"] = build_consts(inp, b)
        maps.append(m)
    return maps


def kernel(**inputs):
    inp = {k: np.asarray(v) for k, v in inputs.items()}
    nb, S, _ = inp["x"].shape
    CT = inp["ctx"].shape[1]
    nc = _get_nc(S, CT)
    maps = make_in_maps(inp, nb)
    res = run_bass_kernel_spmd(nc, maps, core_ids=list(range(nb)))
    out = np.stack([np.ascontiguousarray(r["outT"].T) for r in res.results], axis=0)
    return out.astype(np.float32)
```

```python
import math
from contextlib import ExitStack
import numpy as np
import concourse.bass as bass
import concourse.mybir as mybir
from concourse.bass_utils import run_bass_kernel_spmd

F32 = mybir.dt.float32
BF16 = mybir.dt.bfloat16
AF = mybir.ActivationFunctionType
ALU = mybir.AluOpType

D = 1024
KC = 8
PROJ = 3088
DFF = 2816
DFE = 3584
NE = 8
EPS = 1e-6
NEG = -30000.0
EPOCH = 30000
NDMA_SEMS = 32
NSW_SEMS = 12


class Prog:
    ENGS = ("pe", "act", "dve", "pool", "sp")

    def __init__(self, nc, stack):
        self.nc = nc
        self.stack = stack
        self.streams = {e: [] for e in self.ENGS}
        self.cnt = {e: 0 for e in self.ENGS}
        self.esems = {e: [] for e in self.ENGS}
        self.seen = {e: {} for e in self.ENGS}
        self.last_w = {}
        self.readers = {}
        self.dma_sems = {"hw": [stack.enter_context(nc.semaphore(f"dq{i}")) for i in range(NDMA_SEMS)],
                         "sw": [stack.enter_context(nc.semaphore(f"dw{i}")) for i in range(NSW_SEMS)]}
        self.ndma = {"hw": 0, "sw": 0}

    def _eng_sem(self, e, epoch):
        while len(self.esems[e]) <= epoch:
            self.esems[e].append(self.stack.enter_context(self.nc.semaphore(f"s_{e}_{len(self.esems[e])}")))
        return self.esems[e][epoch]

    def _need(self, e, tok, waits):
        if tok is None:
            return
        sem, val, src = tok
        if src == e and e == "pe":
            return
        sid = id(sem)
        if self.seen[e].get(sid, 0) >= val:
            return
        self.seen[e][sid] = val
        waits.append((sem, val))

    @staticmethod
    def _x(reads, writes):
        r, w = [], list(writes)
        for k in reads:
            if isinstance(k, tuple) and k and k[0] == "PS":
                w.append(k)
            else:
                r.append(k)
        return r, w

    def _deps(self, e, reads, writes):
        reads, writes = self._x(reads, writes)
        waits = []
        for k in reads:
            self._need(e, self.last_w.get(k), waits)
        for k in writes:
            self._need(e, self.last_w.get(k), waits)
            for t in self.readers.get(k, ()):
                if t[2] == e and t[3] is False:
                    continue
                self._need(e, t[:3], waits)
        return waits

    def _commit(self, tok, reads, writes, is_dma):
        reads, writes = self._x(reads, writes)
        for k in reads:
            self.readers.setdefault(k, []).append(tok + (is_dma,))
        for k in writes:
            self.last_w[k] = tok
            self.readers[k] = []

    def op(self, e, fn, reads=(), writes=()):
        waits = self._deps(e, reads, writes)
        n = self.cnt[e]
        epoch, idx = divmod(n, EPOCH)
        sem = self._eng_sem(e, epoch)
        self.cnt[e] = n + 1
        tok = (sem, idx + 1, e)
        self.streams[e].append((waits, fn, sem, 1))
        self._commit(tok, reads, writes, False)

    def dma(self, e, out, in_, reads=(), writes=()):
        waits = self._deps(e, reads, writes)
        kind = "sw" if e == "pool" else "hw"
        pool_ = self.dma_sems[kind]
        n = self.ndma[kind]
        self.ndma[kind] += 1
        slot, rnd = n % len(pool_), n // len(pool_)
        sem = pool_[slot]
        if rnd > 0:
            self._need(e, (sem, 16 * rnd, None), waits)
        tok = (sem, 16 * (rnd + 1), None)
        self.streams[e].append((waits, lambda eng: eng.dma_start(out=out, in_=in_), sem, 16))
        self._commit(tok, reads, writes, True)
        return tok

    def barrier(self):
        toks = []
        for e in self.ENGS:
            n = self.cnt[e]
            if n:
                epoch, idx = divmod(n - 1, EPOCH)
                toks.append((self.esems[e][epoch], idx + 1, None))
        for kind, pool_ in self.dma_sems.items():
            nd = self.ndma[kind]
            for n in range(max(0, nd - len(pool_)), nd):
                toks.append((pool_[n % len(pool_)], 16 * (n // len(pool_) + 1), None))
        for e in self.ENGS:
            waits = []
            for t in toks:
                self._need(e, t, waits)
            if waits:
                self.streams[e].append((waits, None, None, 0))

    def final_wait(self, e, toks):
        waits = []
        for t in toks:
            self._need(e, t, waits)
        self.streams[e].append((waits, None, None, 0))

    def emit(self):
        nc = self.nc
        with nc.Block() as block:
            def run(eng, stream):
                for waits, fn, sem, inc in stream:
                    for (s, v) in waits:
                        eng.wait_ge(s, v)
                    if fn is not None:
                        fn(eng).then_inc(sem, inc)

            @block.tensor
            def _(eng):
                run(eng, self.streams["pe"])

            @block.scalar
            def _(eng):
                run(eng, self.streams["act"])

            @block.vector
            def _(eng):
                run(eng, self.streams["dve"])

            @block.gpsimd
            def _(eng):
                run(eng, self.streams["pool"])

            @block.sync
            def _(eng):
                run(eng, self.streams["sp"])


def _cmap():
    off = {}
    o = 0

    def add(name, n):
        nonlocal o
        off[name] = (o, n)
        o += n

    add("ident", 128)
    add("Lf", 128)
    add("Lb", 128)
    add("ones", 128)
    for nm in ("nm_f_incl", "nm_f_strict", "nm_b_incl", "nm_b_strict"):
        add(nm, 128)
    add("sel", 1024)
    add("gm", 1024)
    add("D2", 128)
    for sz in (2, 4, 8, 16, 32, 64):
        add(f"B{sz}", 128)
    for l in range(2):
        add(f"n1g{l}", 8)
        add(f"n2g{l}", 8)
        add(f"bmod{l}", 48)
        add(f"convw{l}", 60)
        add(f"ong{l}", 1)
        add(f"alog{l}", 1)
        add(f"dtb{l}", 1)
        add(f"sgug{l}", 512)
        add(f"bsrow{l}", 512)
    add("fing", 8)
    add("cvec", 16)
    add("rbrow", 8)
    return off, o


CMAP, NCST = _cmap()


def build_consts(inp, b):
    c = np.zeros((128, NCST), np.float32)

    def put(name, arr):
        o, n = CMAP[name]
        arr = np.asarray(arr, np.float32)
        assert arr.shape[1] == n, (name, arr.shape, n)
        c[:arr.shape[0], o:o + n] = arr

    p = np.arange(128)[:, None]
    f = np.arange(128)[None, :]
    put("ident", (p == f))
    put("Lf", (f >= p))
    put("Lb", (f <= p))
    put("ones", np.ones((128, 128)))
    put("nm_f_incl", np.where(f >= p, 0.0, NEG))
    put("nm_f_strict", np.where(f > p, 0.0, NEG))
    put("nm_b_incl", np.where(f <= p, 0.0, NEG))
    put("nm_b_strict", np.where(f < p, 0.0, NEG))
    gm = np.zeros((8, 2, 4, 128), np.float32)
    for h_ in range(4):
        gm[h_, 0, h_, :] = 1.0
        gm[4 + h_, 1, h_, :] = 1.0
    put("gm", gm.reshape(8, 1024))
    put("D2", (p // 2 == f // 2))
    for sz in (2, 4, 8, 16, 32, 64):
        put(f"B{sz}", (p // (2 * sz) == f // (2 * sz)) & (p // sz != f // sz))
    sel = np.zeros((8, 8, 128), np.float32)
    for r in range(8):
        sel[r, r, :] = 1.0
    put("sel", sel.reshape(8, 1024))
    fm = lambda v: np.asarray(v, np.float32).reshape(-1, 128).T
    for l in range(2):
        put(f"n1g{l}", fm(inp["norm1_g"][l]))
        put(f"n2g{l}", fm(inp["norm2_g"][l]))
        put(f"bmod{l}", fm(inp["b_mod"][l]))
        cw = np.asarray(inp["conv_w"][l], np.float32)
        put(f"convw{l}", cw.T.reshape(12, 128, 5).transpose(1, 0, 2).reshape(128, 60))
        put(f"ong{l}", np.asarray(inp["o_norm_g"][l], np.float32).reshape(128, 1))
        al = np.zeros((16, 1), np.float32)
        db = np.zeros((16, 1), np.float32)
        for d_ in range(2):
            al[d_ * 8:d_ * 8 + 4, 0] = inp["a_log"][l][d_]
            db[d_ * 8:d_ * 8 + 4, 0] = inp["dt_bias"][l][d_]
        put(f"alog{l}", al)
        put(f"dtb{l}", db)
        put(f"sgug{l}", np.broadcast_to(np.asarray(inp["sgu_norm_g"][l], np.float32)[None, :], (128, 512)))
        put(f"bsrow{l}", np.broadcast_to(np.asarray(inp["b_s"][l], np.float32).reshape(1, 512), (128, 512)))
    put("fing", fm(inp["final_g"]))
    cv = np.stack([fm(inp["c"][b]), fm(inp["c_ctx"])], axis=-1)
    put("cvec", cv.reshape(128, 16))
    put("rbrow", np.broadcast_to(np.asarray(inp["router_b"][0], np.float32)[None, :], (128, 8)))
    return c


def grid_pos_embed_T(seq, dim, grid_w=64):
    rows = seq // grid_w
    row = np.broadcast_to(np.arange(rows, dtype=np.float32)[:, None], (rows, grid_w)).reshape(-1)
    col = np.broadcast_to(np.arange(grid_w, dtype=np.float32)[None, :], (rows, grid_w)).reshape(-1)
    quarter = dim // 4
    omega = (1.0 / (10000.0 ** (np.arange(quarter, dtype=np.float32) / np.float32(quarter)))).astype(np.float32)

    def enc(pv):
        ang = (pv[:, None] * omega[None, :]).astype(np.float32)
        return np.concatenate([np.sin(ang), np.cos(ang)], axis=-1)

    return np.ascontiguousarray(np.concatenate([enc(row), enc(col)], axis=-1).astype(np.float32).T)


def mm(out, lhsT, rhs, start=True, stop=True):
    return lambda e: e.matmul(out, lhsT=lhsT, rhs=rhs, start=start, stop=stop)


def act(out, in_, func, **kw):
    return lambda e: e.activation(out=out, in_=in_, func=func, **kw)


def tt(out, in0, in1, op):
    return lambda e: e.tensor_tensor(out=out, in0=in0, in1=in1, op=op)


def ts(out, in0, s1, op0, s2=None, op1=None):
    if op1 is None:
        return lambda e: e.tensor_scalar(out=out, in0=in0, scalar1=s1, scalar2=None, op0=op0)
    return lambda e: e.tensor_scalar(out=out, in0=in0, scalar1=s1, scalar2=s2, op0=op0, op1=op1)


def stt(out, in0, scalar, in1, op0, op1):
    return lambda e: e.scalar_tensor_tensor(out=out, in0=in0, scalar=scalar, in1=in1, op0=op0, op1=op1)


def cp(out, in_):
    return lambda e: e.tensor_copy(out=out, in_=in_)


def ms(ap, val):
    return lambda e: e.memset(ap, val)


def tr(out, in_, ident):
    return lambda e: e.transpose(out=out, in_=in_, identity=ident)


def red(out, in_, op):
    return lambda e: e.tensor_reduce(out=out, in_=in_, axis=mybir.AxisListType.X, op=op)


class Builder:
    def __init__(self, S, CT, debug=False):
        self.S, self.CT, self.debug = S, CT, debug
        self.NT = min(1024, S)

    def f32(self, name, cols):
        self.alog = getattr(self, "alog", [])
        self.alog.append((name, self.apos, cols, "f32"))
        a = self.arena[:, self.apos:self.apos + cols]
        self.apos += cols
        assert self.apos <= self.AW, (name, self.apos, self.AW)
        return a

    def b16(self, name, cols):
        w = (cols + 1) // 2
        self.alog = getattr(self, "alog", [])
        self.alog.append((name, self.apos, cols, "b16"))
        a = self.arena[:, self.apos:self.apos + w].bitcast(BF16)
        self.apos += w
        assert self.apos <= self.AW, (name, self.apos, self.AW)
        return a[:, 0:cols]

    def cs(self, name, n0=0, n1=None, rows=128):
        o, n = CMAP[name]
        if n1 is None:
            n1 = n
        return self.cst[0:rows, o + n0:o + n1]

    def _bank(self):
        i = self._pb % len(self.banks)
        self._pb += 1
        return self.banks[i], ("PS", i)

    def ps_half(self):
        bk, k = self._bank()
        return bk[:, 0:256], k

    def ps_q(self):
        bk, k = self._bank()
        return bk[:, 0:128], k

    def ps_t(self):
        i = self._pt % len(self.pt16)
        self._pt += 1
        return self.pt16[i][:, 0:128], ("PS", "t", i)

    def ps_full(self, lo=0, n=7):
        bk, k = self._bank()
        return bk[:, :], k

    def build(self):
        S, CT = self.S, self.CT
        nc = bass.Bass("TRN2", target_bir_lowering=False)
        self.nc = nc
        din = lambda name, shape, dt=F32: nc.dram_tensor(name, shape, dt, kind="ExternalInput").ap()
        dscr = lambda name, shape, dt=F32: nc.dram_tensor(
            name, shape, dt, kind="ExternalOutput" if self.debug else "Internal").ap()
        self.xT = din("xT", [D, S])
        self.ctxT = din("ctxT", [D, CT])
        self.posT = din("posT", [D, S])
        self.cst_d = din("cst", [128, NCST])
        self.w_mod = [din(f"w_mod{l}", [D, 6 * D]) for l in range(2)]
        self.w_in = [din(f"w_in{l}", [D, PROJ]) for l in range(2)]
        self.w_out = [din(f"w_out{l}", [D, D]) for l in range(2)]
        self.wsT = din("wsT", [2, 128, 512])
        self.ffn_w1 = din("ffn_w1", [D, DFF])
        self.ffn_w3 = din("ffn_w3", [D, DFF])
        self.ffn_w2 = din("ffn_w2", [DFF, D])
        self.exp_w1 = din("exp_w1", [NE, D, DFE])
        self.exp_w3 = din("exp_w3", [NE, D, DFE])
        self.exp_w2 = din("exp_w2", [NE, DFE, D])
        self.router_w = din("router_w", [D, NE])
        self.outT = nc.dram_tensor("outT", [D, S], F32, kind="ExternalOutput").ap()
        self.xa = dscr("xa", [D, S])
        self.xb = dscr("xb", [D, S])
        self.ca = dscr("ca", [D, CT])
        self.cb = dscr("cb", [D, CT])
        self.of = dscr("of", [512, CT + S])
        self.qkvs = dscr("qkvs", [(CT + S) // 128, 128, 1536], BF16)
        self.tms = dscr("tms", [(CT + S) // 128, 128, 48])
        self.w_in_b = [dscr(f"w_in_b{l}", [128, KC, PROJ], BF16) for l in range(2)]
        self.w_out_b = [dscr(f"w_out_b{l}", [128, KC, D], BF16) for l in range(2)]
        self.f13b = dscr("f13b", [6, 128, 2, KC, 512], BF16)
        self.f2b = dscr("f2b", [4, 128, 22, 256], BF16)
        self.e13b = dscr("e13b", [NE, 7, 128, 2, KC, 512], BF16)
        self.e2b = dscr("e2b", [NE, 4, 128, 28, 256], BF16)

        with ExitStack() as st:
            self.P = P = Prog(nc, st)
            self.AW = 51000
            self.arena = st.enter_context(nc.sbuf_tensor("arena", [128, self.AW], F32))
            self.banks = [st.enter_context(nc.psum_tensor(f"bank{i}", [128, 512], F32)) for i in range(6)]
            self.pt16 = [st.enter_context(nc.psum_tensor(f"pt16_{i}", [128, 1024], BF16)) for i in range(2)]
            self._pb = self._pt = 0
            self.apos = 0
            self.setup()
            self.persist_end = self.apos
            for l in range(2):
                self.apos = self.persist_end
                P.barrier()
                self.sweep_phase(l)
                self.apos = self.persist_end
                P.barrier()
                self.mlp_phase(l)
            P.barrier()
            P.final_wait("sp", self.out_toks)
            P.emit()
        return nc

    def setup(self):
        P, nc = self.P, self.nc
        S, CT = self.S, self.CT
        self.out_toks = []
        self.cst = self.f32("cst", NCST)
        P.dma("sp", self.cst, self.cst_d, writes=["cst"])
        for l in range(2):
            P.dma("pool", self.w_in_b[l], self.w_in[l].rearrange("(kc p) n -> p kc n", p=128), writes=[("w_in_b", l)])
            P.dma("pool", self.w_out_b[l], self.w_out[l].rearrange("(kc p) n -> p kc n", p=128), writes=[("w_out_b", l)])
        self.ident16 = self.b16("ident16", 128)
        self.ones16 = self.b16("ones16", 128)
        self.nm16 = {}
        P.op("dve", cp(self.ident16, self.cs("ident")), reads=["cst"], writes=["ident16"])
        P.op("dve", ms(self.ones16, 1.0), writes=["ones16"])
        for nm in ("nm_f_incl", "nm_f_strict", "nm_b_incl", "nm_b_strict", "D2", "B2", "B4", "B8", "B16", "B32", "B64"):
            t = self.b16(nm, 128)
            self.nm16[nm] = t
            P.op("dve", cp(t, self.cs(nm)), reads=["cst"], writes=[nm])
        self.wsT16 = self.b16("wsT16", 1024)
        P.dma("pool", self.wsT16.rearrange("p (l n) -> p l n", l=2), self.wsT.rearrange("l q n -> q l n"),
              writes=["wsT16"])
        self.rw32 = self.f32("rw32", 64)
        P.dma("sp", self.rw32.rearrange("p (k n) -> p k n", k=8), self.router_w.rearrange("(k p) n -> p k n", p=128),
              writes=["rw32"])
        s32 = self.f32("s32", 16)
        P.op("act", act(s32, self.cs("cvec"), AF.Silu), reads=["cst"], writes=["s32"])
        s3 = s32.rearrange("p (k s) -> p k s", k=8)
        modvs = [self.f32(f"modv{l}", 96) for l in range(2)]
        self.vec = {}
        for l in range(2):
            for s in range(2):
                self.vec[(l, s)] = {nm: self.f32(nm, 8) for nm in ("gs1", "sh1", "g1", "gs2", "sh2", "g2")}
            self.vec[(l, "nega")] = self.f32("nega", 1)
        keep = self.apos
        wmb = [self.f32(f"wmb{i}", KC * 512).rearrange("p (k n) -> p k n", k=8) for i in range(2)]
        nblk = 0
        for l in range(2):
            modv = modvs[l]
            for blk in range(12):
                wb = wmb[nblk % 2]
                key = ("wmb", nblk % 2)
                nblk += 1
                P.dma("sp", wb, self.w_mod[l].rearrange("(k p) n -> p k n", p=128)[:, :, blk * 512:(blk + 1) * 512],
                      writes=[key])
                for j in range(4):
                    oc = blk * 4 + j
                    pq, pk = self.ps_q()
                    for kc in range(KC):
                        P.op("pe", mm(pq[:, 0:2], wb[:, kc, j * 128:(j + 1) * 128], s3[:, kc, :], kc == 0, kc == KC - 1),
                             reads=[key, "s32"], writes=[pk])
                    P.op("dve", ts(modv[:, oc * 2:oc * 2 + 2], pq[:, 0:2], self.cs(f"bmod{l}", oc, oc + 1), ALU.add),
                         reads=[pk, "cst"], writes=[("modv", l)])
            mv = modv.rearrange("p (o s) -> p o s", s=2)
            for s in range(2):
                v = self.vec[(l, s)]
                for nm, part, ng in (("1", 0, f"n1g{l}"), ("2", 3, f"n2g{l}")):
                    P.op("dve", stt(v["gs" + nm], mv[:, (part + 1) * 8:(part + 2) * 8, s], 1.0, self.cs(ng), ALU.add, ALU.mult),
                         reads=[("modv", l), "cst"], writes=[("vec", l, s)])
                    P.op("dve", cp(v["sh" + nm], mv[:, part * 8:(part + 1) * 8, s]), reads=[("modv", l)], writes=[("vec", l, s)])
                    P.op("dve", cp(v["g" + nm], mv[:, (part + 2) * 8:(part + 3) * 8, s]), reads=[("modv", l)], writes=[("vec", l, s)])
            na = self.vec[(l, "nega")]
            P.op("act", act(na[0:16, :], self.cs(f"alog{l}", rows=16), AF.Exp), reads=["cst"], writes=[("nega", l)])
            P.op("dve", ts(na[0:16, :], na[0:16, :], -1.0, ALU.mult), reads=[("nega", l)], writes=[("nega", l)])
        TT = min(512, S)
        xt = [self.f32("xt0", KC * TT).rearrange("p (k n) -> p k n", k=8) for _ in range(2)]
        pt = [self.f32("pt0", KC * TT).rearrange("p (k n) -> p k n", k=8) for _ in range(2)]
        r3 = lambda ap: ap.rearrange("(k p) t -> p k t", p=128)
        for i in range(S // TT):
            b = i % 2
            sl = slice(i * TT, (i + 1) * TT)
            P.dma("sp", xt[b], r3(self.xT)[:, :, sl], writes=[("x0", b)])
            P.dma("sp", pt[b], r3(self.posT)[:, :, sl], writes=[("p0", b)])
            P.op("dve", tt(xt[b], xt[b], pt[b], ALU.add), reads=[("x0", b), ("p0", b)], writes=[("x0", b)])
            P.dma("sp", r3(self.xa)[:, :, sl], xt[b], reads=[("x0", b)], writes=["xa"])
        P.dma("sp", pt[0][:, :, 0:CT], r3(self.ctxT), writes=[("p0", 0)])
        P.dma("sp", r3(self.ca), pt[0][:, :, 0:CT], reads=[("p0", 0)], writes=["ca"])
        self.apos = keep
        for blk in range(6):
            fw = 512 if blk < 5 else 256
            for j, w in enumerate((self.ffn_w1, self.ffn_w3)):
                P.dma("pool", self.f13b[blk, :, j, :, 0:fw],
                      w.rearrange("(k p) f -> p k f", p=128)[:, :, blk * 512:blk * 512 + fw], writes=["f13b"])
        for db in range(4):
            P.dma("pool", self.f2b[db], self.ffn_w2.rearrange("(fc p) c -> p fc c", p=128)[:, :, db * 256:(db + 1) * 256],
                  writes=["f2b"])
        for ex in range(NE):
            for blk in range(7):
                for j, w in enumerate((self.exp_w1, self.exp_w3)):
                    P.dma("pool", self.e13b[ex, blk, :, j],
                          w[ex].rearrange("(k p) f -> p k f", p=128)[:, :, blk * 512:(blk + 1) * 512], writes=[("e13b", ex)])
            for db in range(4):
                P.dma("pool", self.e2b[ex, db],
                      self.exp_w2[ex].rearrange("(fc p) c -> p fc c", p=128)[:, :, db * 256:(db + 1) * 256],
                      writes=[("e2b", ex)])

    def sweep_phase(self, l):
        P = self.P
        S, CT = self.S, self.CT
        last = (l == 1)
        xin, xout = (self.xa, self.xb)
        cin, cout = (self.ca, self.cb)
        r3 = lambda ap: ap.rearrange("(k p) t -> p k t", p=128)
        TW, XW = 256, 260
        w_in16 = self.b16("w_in16", KC * PROJ).rearrange("p (k n) -> p k n", k=8)
        P.dma("sp", w_in16, self.w_in_b[l], reads=[("w_in_b", l)], writes=["w_in16"])
        cw = self.cs(f"convw{l}").rearrange("p (c j) -> p c j", j=5)
        nega = self.vec[(l, "nega")]
        dtb = self.cs(f"dtb{l}", rows=16)
        NB = 2
        A = lambda name, cols, n=NB: [self.f32(name, cols) for _ in range(n)]
        B = lambda name, cols, n=NB: [self.b16(name, cols) for _ in range(n)]
        xt = [a.rearrange("p (k n) -> p k n", k=8) for a in A("xt", KC * XW)]
        sq16 = [a.rearrange("p (k n) -> p k n", k=8) for a in B("sq16", KC * XW, 1)]
        rinv = A("rinv", XW, 1)
        tmp = [a.rearrange("p (k n) -> p k n", k=8) for a in A("tmp", KC * XW, 1)]
        h16 = [a.rearrange("p (k n) -> p k n", k=8) for a in B("h16", KC * XW, 1)]
        qkv16 = B("qkv16", 1536)
        q16 = [a[:, 0:512].rearrange("p (c n) -> p c n", c=4) for a in qkv16]
        k16 = [a[:, 512:1024].rearrange("p (c n) -> p c n", c=4) for a in qkv16]
        v16 = [a[:, 1024:1536].rearrange("p (c n) -> p c n", c=4) for a in qkv16]
        tm = A("tm", 48)
        gcs = A("gcs", 8)
        sm = A("sm", 40)
        rbT = A("rbT", 128)
        rbig = A("rbig", 1024)
        o32 = [a.rearrange("p (c n) -> p c n", c=4) for a in A("o32", 512)]
        HB = 2
        Eq = A("Eq", 128, 4); Ea = A("Ea", 128, 4); EG = A("EG", 128, 4)
        M16 = B("M16", 128, HB); qkT16 = B("qkT16", 128, HB)
        Pb = [B("Pb", 128, HB) for _ in range(2)]
        Qb = [B("Qb", 128, HB) for _ in range(2)]
        Rb = [B("Rb", 128, HB) for _ in range(2)]
        kbg16 = B("kbg16", 128, HB); kdec16 = B("kdec16", 128, HB); vb16 = B("vb16", 128, HB)
        u32 = A("u32", 128, HB); wT16 = B("wT16", 128, HB); qd16 = B("qd16", 128, HB); vn16 = B("vn16", 128, HB)
        S32 = A("S32", 128, 4); S16 = B("S16", 128, 4)
        ong = self.cs(f"ong{l}")
        sgug = self.cs(f"sgug{l}")
        bsrow = self.cs(f"bsrow{l}").rearrange("p (g n) -> p g n", g=4)
        mark = self.apos
        seqs = [("c", cin, cout, CT, 1, 0), ("x", xin, xout, S, 0, CT)]

        for dr in (0, 1):
            full = (dr == 1)
            self.apos = mark
            P.barrier()
            if not full:
                pq = [a.rearrange("p (c n) -> p c n", c=12) for a in A("pq", 12 * XW, 1)]
                cy = [a.rearrange("p (c n) -> p c n", c=12) for a in A("cy", 12 * TW, 1)]
                sqk16 = B("sqk16", 8 * TW, 1)
                rqk = A("rqk", 8 * TW, 1)
                gt = A("gt", TW * 6, 1)
            else:
                w_out16 = self.b16("w_out16", KC * D).rearrange("p (k n) -> p k n", k=8)
                P.dma("sp", w_out16, self.w_out_b[l], reads=[("w_out_b", l)], writes=["w_out16"])
                zs = [a.rearrange("p (c n) -> p c n", c=4) for a in A("zs", 4 * TW, 1)]
                gu = [a.rearrange("p (c n) -> p c n", c=4) for a in A("gu", 4 * TW, 1)]
                gv = A("gv", 512, 1)
                gvn16 = B("gvn16", 512, 1)
                st6 = A("st6", 8, 1)
                off = [a.rearrange("p (c n) -> p c n", c=4) for a in A("off", 512)]
                osq16 = B("osq16", 512, 1)
                orinv = A("orinv", 512, 1)
                cat16 = self.b16("cat16", KC * TW).rearrange("p (k n) -> p k n", k=8)
                mixt = A("mixt", 128, 1)
                xn = self.f32("xn", KC * TW).rearrange("p (k n) -> p k n", k=8)
            nm_incl = self.nm16["nm_f_incl" if dr == 0 else "nm_b_incl"]
            nm_strict = self.nm16["nm_f_strict" if dr == 0 else "nm_b_strict"]
            Lc = self.cs("Lf" if dr == 0 else "Lb")
            for h in range(4):
                P.op("dve", ms(S32[h], 0.0), writes=[("S32", h)])
                P.op("dve", ms(S16[h], 0.0), writes=[("S16", h)])
            tiles = []
            for sq in seqs:
                nst = sq[3] // TW
                order = range(nst) if dr == 0 else range(nst - 1, -1, -1)
                tiles += [(sq, st) for st in order]

            def load(i):
                (name, xi, xo, L, strm, ofo), st = tiles[i]
                sb = i % 2
                a0, a1 = max(0, st * TW - 2), min(L, st * TW + TW + 2)
                c0 = a0 - (st * TW - 2)
                if c0 > 0:
                    P.op("pool", ms(xt[sb][:, :, 0:2], 0.0), writes=[("xt", sb)])
                if a1 < st * TW + TW + 2:
                    P.op("pool", ms(xt[sb][:, :, XW - 2:XW], 0.0), writes=[("xt", sb)])
                P.dma("sp", xt[sb][:, :, c0:c0 + (a1 - a0)], r3(xi)[:, :, a0:a1], reads=[("xsrc", name)], writes=[("xt", sb)])

            load(0)
            for i in range(len(tiles)):
                if i + 1 < len(tiles):
                    load(i + 1)
                (name, xi, xo, L, strm, ofo), st = tiles[i]
                sb = i % 2
                V = self.vec[(l, strm)]
                do_out = full and not (last and name == "c")
                halves = (0, 1) if dr == 0 else (1, 0)
                if full:
                    for hf in halves:
                        t = st * 2 + hf
                        ti = ofo // 128 + t
                        P.dma("sp", off[hf], self.of.rearrange("(h p) t -> p h t", p=128)[:, :, ofo + t * 128: ofo + (t + 1) * 128],
                              reads=[("of", name, t)], writes=[("off", hf)])
                        P.dma("sp", qkv16[hf], self.qkvs[ti], reads=[("qkvs", ti)], writes=[("q16", hf), ("k16", hf), ("v16", hf)])
                        P.dma("sp", tm[hf], self.tms[ti], reads=[("tms", ti)], writes=[("tm", hf)])
                if (not full) or do_out:
                    P.op("act", act(sq16[0], xt[sb], AF.Square), reads=[("xt", sb)], writes=["sq16"])
                    ps, pk = self.ps_full()
                    for kc in range(KC):
                        P.op("pe", mm(ps[:, 0:XW], self.ones16, sq16[0][:, kc, :], kc == 0, kc == KC - 1),
                             reads=["sq16", "ones16"], writes=[pk])
                    P.op("act", act(rinv[0], ps[:, 0:XW], AF.Ln, bias=EPS, scale=1.0 / D), reads=[pk], writes=["rinv"])
                    P.op("act", act(rinv[0], rinv[0], AF.Exp, scale=-0.5), reads=["rinv"], writes=["rinv"])
                    for kc in range(KC):
                        P.op("dve", stt(tmp[0][:, kc, :], xt[sb][:, kc, :], V["gs1"][:, kc:kc + 1], rinv[0], ALU.mult, ALU.mult),
                             reads=[("xt", sb), "rinv", ("vec", l, strm)], writes=[("tmp", kc)])
                        P.op("act", act(h16[0][:, kc, :], tmp[0][:, kc, :], AF.Identity, bias=V["sh1"][:, kc:kc + 1], scale=1.0),
                             reads=[("tmp", kc), ("vec", l, strm)], writes=["h16"])
                    ocs = (list(range(12)) + [16]) if not full else [12, 13, 14, 15, 17, 18, 19, 20]
                    for oc in ocs:
                        n0 = {16: 2048}.get(oc, oc * 128 if oc < 16 else 2064 + (oc - 17) * 128)
                        rows = 16 if oc == 16 else 128
                        ps, pk = self.ps_full()
                        for kc in range(KC):
                            P.op("pe", mm(ps[0:rows, 0:XW], w_in16[:, kc, n0:n0 + rows], h16[0][:, kc, :], kc == 0, kc == KC - 1),
                                 reads=["w_in16", "h16"], writes=[pk])
                        if oc < 12:
                            P.op("act", act(pq[0][:, oc, :], ps[:, 0:XW], AF.Copy), reads=[pk], writes=[("pq", oc)])
                        elif oc < 16:
                            P.op("act", act(zs[0][:, oc - 12, :], ps[:, 2:2 + TW], AF.Silu), reads=[pk], writes=[("zs", oc - 12)])
                        elif oc == 16:
                            g = gt[0].rearrange("p (r n) -> p r n", r=6)
                            P.op("dve", ts(g[0:16, 0, :], ps[0:16, 2:2 + TW], dtb, ALU.add), reads=[pk, "cst"], writes=["gt0"])
                            P.op("act", act(g[0:16, 4, :], ps[0:16, 2:2 + TW], AF.Sigmoid), reads=[pk], writes=["gt4"])
                        else:
                            P.op("act", act(gu[0][:, oc - 17, :], ps[:, 2:2 + TW], AF.Gelu_apprx_tanh), reads=[pk], writes=[("gu", oc - 17)])
                if not full:
                    pqk = [("pq", oc) for oc in range(12)]
                    if st == 0:
                        P.op("pool", ms(pq[0][:, :, 0:2], 0.0), reads=pqk, writes=pqk)
                    if st == L // TW - 1:
                        P.op("pool", ms(pq[0][:, :, XW - 2:XW], 0.0), reads=pqk, writes=pqk)
                    for oc in range(12):
                        P.op("act", act(cy[0][:, oc, :], pq[0][:, oc, 0:TW], AF.Copy, scale=cw[:, oc, 0:1]),
                             reads=[("pq", oc), "cst"], writes=[("cy", oc), "sl32"])
                        for j in range(1, 5):
                            P.op("dve", stt(cy[0][:, oc, :], pq[0][:, oc, j:j + TW], cw[:, oc, j:j + 1], cy[0][:, oc, :], ALU.mult, ALU.add),
                                 reads=[("pq", oc), ("cy", oc), "cst"], writes=[("cy", oc)])
                    cyk = [("cy", oc) for oc in range(12)]
                    P.op("act", act(cy[0], cy[0], AF.Silu), reads=cyk, writes=["sl32"] + cyk)
                    sl_flat = cy[0].rearrange("p c n -> p (c n)")
                    P.op("act", act(sqk16[0], sl_flat[:, 0:8 * TW], AF.Square), reads=["sl32"], writes=["sqk16"])
                    for q4 in range(4):
                        ps, pk = self.ps_full()
                        P.op("pe", mm(ps, self.ones16, sqk16[0][:, q4 * 512:(q4 + 1) * 512]), reads=["sqk16", "ones16"], writes=[pk])
                        P.op("act", act(rqk[0][:, q4 * 512:(q4 + 1) * 512], ps, AF.Ln, bias=EPS, scale=1.0), reads=[pk], writes=[("rqk", q4)])
                        P.op("act", act(rqk[0][:, q4 * 512:(q4 + 1) * 512], rqk[0][:, q4 * 512:(q4 + 1) * 512], AF.Exp, scale=-0.5),
                             reads=[("rqk", q4)], writes=[("rqk", q4)])
                    rq3 = rqk[0].rearrange("p (c n) -> p c n", c=8)
                    rqks = [("rqk", q4) for q4 in range(4)]
                    g = gt[0].rearrange("p (r n) -> p r n", r=6)
                    R16 = lambda r: g[0:16, r, :]
                    P.op("act", act(R16(1), R16(0), AF.Abs), reads=["gt0"], writes=["gt1"])
                    P.op("act", act(R16(1), R16(1), AF.Exp, scale=-1.0), reads=["gt1"], writes=["gt1"])
                    P.op("act", act(R16(1), R16(1), AF.Ln, bias=1.0, scale=1.0), reads=["gt1"], writes=["gt1"])
                    P.op("dve", stt(R16(2), R16(0), 0.0, R16(1), ALU.max, ALU.add), reads=["gt0", "gt1"], writes=["gt2"])
                    P.op("dve", ts(R16(3), R16(2), nega[0:16, :], ALU.mult), reads=["gt2", ("nega", l)], writes=["gt3"])
                    P.op("dve", ts(R16(5), R16(4), 1e-30, ALU.max), reads=["gt4"], writes=["gt5"])
                    P.op("act", act(R16(5), R16(5), AF.Ln), reads=["gt5"], writes=["gt5"])
                for hf in halves:
                    t = st * 2 + hf
                    b = hf
                    hsl = slice(hf * 128, (hf + 1) * 128)
                    if not full:
                        P.op("dve", stt(q16[b], cy[0][:, 0:4, hsl], 128.0 ** -0.5, rq3[:, 0:4, hsl], ALU.mult, ALU.mult),
                             reads=["sl32"] + rqks, writes=[("q16", b)])
                        P.op("dve", tt(k16[b], cy[0][:, 4:8, hsl], rq3[:, 4:8, hsl], ALU.mult), reads=["sl32"] + rqks, writes=[("k16", b)])
                        P.op("pool", cp(v16[b], cy[0][:, 8:12, hsl]), reads=["sl32"], writes=[("v16", b)])
                        psq, pkq = self.ps_q()
                        for j, r in enumerate((3, 4, 5)):
                            P.op("pe", tr(psq[:, j * 16:(j + 1) * 16], R16(r)[:, hsl], self.cs("ident", 0, 16, rows=16)),
                                 reads=[f"gt{r}", "cst"], writes=[pkq])
                        P.op("act", act(tm[b], psq[:, 0:48], AF.Copy), reads=[pkq], writes=[("tm", b)])
                        ti = ofo // 128 + t
                        P.dma("sp", self.qkvs[ti], qkv16[b], reads=[("q16", b), ("k16", b), ("v16", b)], writes=[("qkvs", ti)])
                        P.dma("sp", self.tms[ti], tm[b], reads=[("tm", b)], writes=[("tms", ti)])
                    gcol = tm[b][:, dr * 8: dr * 8 + 4]
                    bcol = tm[b][:, 16 + dr * 8 + 4: 16 + dr * 8 + 8]
                    lcol = tm[b][:, 32 + dr * 8 + 4: 32 + dr * 8 + 8]
                    psq, pkq = self.ps_q()
                    P.op("pe", mm(psq[:, 0:4], Lc, gcol), reads=[("tm", b), "cst"], writes=[pkq])
                    P.op("pe", mm(psq[:, 4:8], self.cs("ones"), gcol), reads=[("tm", b), "cst"], writes=[pkq])
                    P.op("act", act(gcs[b], psq[:, 0:8], AF.Copy), reads=[pkq], writes=[("gcs", b)])
                    smb = sm[b]
                    gc, tot = gcs[b][:, 0:4], gcs[b][:, 4:8]
                    ngc, dd, kd, eg, bge, glast, rbs = (smb[:, 4:8], smb[:, 8:12], smb[:, 12:16], smb[:, 16:20],
                                                        smb[:, 20:24], smb[:, 24:28], smb[:, 28:36])
                    smk = ("sm", b)
                    P.op("dve", cp(rbs[:, 0:4], gc), reads=[("gcs", b)], writes=[smk])
                    P.op("dve", tt(rbs[:, 4:8], gc, lcol, ALU.add), reads=[("gcs", b), ("tm", b)], writes=[smk])
                    P.op("dve", ts(ngc, gc, -1.0, ALU.mult), reads=[("gcs", b)], writes=[smk])
                    P.op("dve", tt(dd, tot, gc, ALU.subtract), reads=[("gcs", b)], writes=[smk])
                    P.op("act", act(kd, dd, AF.Exp), reads=[smk], writes=[smk])
                    P.op("act", act(eg, gc, AF.Exp), reads=[("gcs", b)], writes=[smk])
                    P.op("act", act(glast, tot, AF.Exp), reads=[("gcs", b)], writes=[smk])
                    P.op("dve", tt(bge, eg, bcol, ALU.mult), reads=[smk, ("tm", b)], writes=[smk])
                    pst, pkt = self.ps_q()
                    P.op("pe", tr(pst[0:8, 0:128], rbs, self.cs("ident")), reads=[smk, "cst"], writes=[pkt])
                    P.op("act", act(rbT[b][0:8, :], pst[0:8, 0:128], AF.Copy), reads=[pkt], writes=[("rbT", b)])
                    gm = self.cs("gm", rows=8).rearrange("p (s h n) -> p s h n", s=2, h=4)
                    rb2 = rbig[b].rearrange("p (s h n) -> p s h n", s=2, h=4)
                    for s_ in range(2):
                        for h in range(4):
                            P.op("pool", tt(rb2[0:8, s_, h, :], rbT[b][0:8, :], gm[:, s_, h, :], ALU.mult),
                                 reads=[("rbT", b), "cst"], writes=[("rbig", b)])
                    pGe, kGe = self.ps_full()
                    P.op("pe", mm(pGe, self.cs("ones", rows=8), rbig[b][0:8, 0:512]), reads=[("rbig", b), "cst"], writes=[kGe])
                    pGa, kGa = self.ps_full()
                    P.op("pe", mm(pGa, self.cs("ones", rows=8), rbig[b][0:8, 512:1024]), reads=[("rbig", b), "cst"], writes=[kGa])
                    nmi32 = self.cs("nm_f_incl" if dr == 0 else "nm_b_incl")
                    nms32 = self.cs("nm_f_strict" if dr == 0 else "nm_b_strict")
                    for h in range(4):
                        hs = slice(h * 128, (h + 1) * 128)
                        P.op("dve", tt(Eq[h], pGe[:, hs], nmi32, ALU.add), reads=[kGe, "cst"], writes=[("Eq", h)])
                        P.op("dve", tt(Ea[h], pGa[:, hs], nms32, ALU.add), reads=[kGa, "cst"], writes=[("Ea", h)])
                        P.op("act", act(EG[h], pGe[:, hs], AF.Exp), reads=[kGe], writes=[("EG", h)])
                        P.op("act", act(Eq[h], Eq[h], AF.Exp, bias=ngc[:, h:h + 1], scale=1.0), reads=[("Eq", h), smk], writes=[("Eq", h)])
                        P.op("act", act(Ea[h], Ea[h], AF.Exp, bias=ngc[:, h:h + 1], scale=1.0), reads=[("Ea", h), smk], writes=[("Ea", h)])
                    for h in range(4):
                        hb = h % HB
                        K = lambda nme: (nme, hb)
                        pKK, kKK = self.ps_q()
                        P.op("pe", mm(pKK, k16[b][:, h, :], k16[b][:, h, :]), reads=[("k16", b)], writes=[kKK])
                        pQK, kQK = self.ps_q()
                        P.op("pe", mm(pQK, k16[b][:, h, :], q16[b][:, h, :]), reads=[("k16", b), ("q16", b)], writes=[kQK])
                        P.op("dve", stt(M16[hb], pKK, -1.0, Ea[h], ALU.mult, ALU.mult), reads=[kKK, ("Ea", h)], writes=[K("M16")])
                        P.op("dve", tt(qkT16[hb], pQK, Eq[h], ALU.mult), reads=[kQK, ("Eq", h)], writes=[K("qkT16")])
                        pN, kN = self.ps_t()
                        P.op("pe", (lambda pN, hb: lambda e: e.transpose(out=pN, in_=M16[hb], identity=self.ident16))(pN, hb),
                             reads=[K("M16"), "ident16"], writes=[kN])
                        Nf, Nfk = Qb[0][hb], ("Qb", 0, hb)
                        P.op("act", act(Nf, pN, AF.Copy), reads=[kN], writes=[Nfk])
                        P.op("pool", tt(Pb[0][hb], M16[hb], self.nm16["D2"], ALU.mult), reads=[K("M16"), "D2"], writes=[("Pb", 0, hb)])
                        P.op("pool", tt(Rb[0][hb], Pb[0][hb], self.ident16, ALU.add), reads=[("Pb", 0, hb), "ident16"], writes=[("Rb", 0, hb)])
                        for li, sz in enumerate((2, 4, 8, 16, 32, 64)):
                            src, dst = li % 2, (li + 1) % 2
                            Xc, Xck = Rb[src][hb], ("Rb", src, hb)
                            Ns, Nsk = Qb[1][hb], ("Qb", 1, hb)
                            P.op("pool", tt(Ns, Nf, self.nm16[f"B{sz}"], ALU.mult), reads=[Nfk, f"B{sz}"], writes=[Nsk])
                            pY, kY = self.ps_q()
                            P.op("pe", mm(pY, Ns, Xc), reads=[Nsk, Xck], writes=[kY])
                            Y16, Yk = Pb[1][hb], ("Pb", 1, hb)
                            P.op("dve", cp(Y16, pY), reads=[kY], writes=[Yk])
                            pXT, kXT = self.ps_t()
                            P.op("pe", tr(pXT, Xc, self.ident16), reads=[Xck, "ident16"], writes=[kXT])
                            XT16, XTk = Pb[0][hb], ("Pb", 0, hb)
                            P.op("act", act(XT16, pXT, AF.Copy), reads=[kXT], writes=[XTk])
                            pR, kR = self.ps_q()
                            P.op("pe", mm(pR, XT16, Y16), reads=[XTk, Yk], writes=[kR])
                            P.op("dve", tt(Rb[dst][hb], pR, Xc, ALU.add), reads=[kR, Xck], writes=[("Rb", dst, hb)])
                        RT, RTk = Rb[0][hb], ("Rb", 0, hb)
                        pkt_, kkt = self.ps_t()
                        P.op("pe", (lambda p_, b, h: lambda e: e.transpose(out=p_, in_=k16[b][:, h, :], identity=self.ident16))(pkt_, b, h),
                             reads=[("k16", b), "ident16"], writes=[kkt])
                        pvt, kvt = self.ps_t()
                        P.op("pe", (lambda p_, b, h: lambda e: e.transpose(out=p_, in_=v16[b][:, h, :], identity=self.ident16))(pvt, b, h),
                             reads=[("v16", b), "ident16"], writes=[kvt])
                        P.op("act", act(kbg16[hb], pkt_, AF.Copy, scale=bge[:, h:h + 1]), reads=[kkt, smk], writes=[K("kbg16")])
                        P.op("act", act(kdec16[hb], pkt_, AF.Copy, scale=kd[:, h:h + 1]), reads=[kkt, smk], writes=[K("kdec16")])
                        P.op("dve", (lambda hb, pvt, h, bcol: lambda e: e.tensor_scalar(
                            out=vb16[hb], in0=pvt, scalar1=bcol[:, h:h + 1], scalar2=None, op0=ALU.mult))(hb, pvt, h, bcol),
                            reads=[kvt, ("tm", b)], writes=[K("vb16")])
                        pu, ku = self.ps_q()
                        P.op("pe", mm(pu, RT, vb16[hb]), reads=[RTk, K("vb16")], writes=[ku])
                        pw, kw = self.ps_q()
                        P.op("pe", mm(pw, kbg16[hb], RT), reads=[RTk, K("kbg16")], writes=[kw])
                        P.op("act", act(u32[hb], pu, AF.Copy), reads=[ku], writes=[K("u32")])
                        P.op("dve", (lambda hb, pw: lambda e: e.tensor_copy(out=wT16[hb], in_=pw))(hb, pw), reads=[kw], writes=[K("wT16")])
                        P.op("pool", tt(qd16[hb], q16[b][:, h, :], EG[h], ALU.mult), reads=[("q16", b), ("EG", h)], writes=[K("qd16")])
                        pws, kws = self.ps_q()
                        P.op("pe", mm(pws, wT16[hb], S16[h]), reads=[K("wT16"), ("S16", h)], writes=[kws])
                        P.op("dve", (lambda hb, pws: lambda e: e.tensor_tensor(out=vn16[hb], in0=u32[hb], in1=pws, op=ALU.subtract))(hb, pws),
                             reads=[kws, K("u32")], writes=[K("vn16")])
                        po, ko = self.ps_q()
                        P.op("pe", mm(po, S16[h], qd16[hb], True, False), reads=[("S16", h), K("qd16")], writes=[ko])
                        P.op("pe", mm(po, vn16[hb], qkT16[hb], False, True), reads=[K("vn16"), K("qkT16")], writes=[ko])
                        psn, ksn = self.ps_q()
                        P.op("pe", mm(psn, kdec16[hb], vn16[hb]), reads=[K("kdec16"), K("vn16")], writes=[ksn])
                        P.op("dve", (lambda h, psn, glast: lambda e: e.scalar_tensor_tensor(
                            out=S32[h], in0=S32[h], scalar=glast[:, h:h + 1], in1=psn, op0=ALU.mult, op1=ALU.add))(h, psn, glast),
                            reads=[ksn, smk, ("S32", h)], writes=[("S32", h)])
                        P.op("act", act(S16[h], S32[h], AF.Copy), reads=[("S32", h)], writes=[("S16", h)])
                        if not full:
                            P.op("act", act(o32[b][:, h, :], po, AF.Copy), reads=[ko], writes=[("o32", b)])
                        else:
                            P.op("dve", (lambda b, h, po: lambda e: e.tensor_tensor(out=o32[b][:, h, :], in0=po, in1=off[b][:, h, :], op=ALU.add))(b, h, po),
                                 reads=[ko, ("off", b)], writes=[("o32", b)])
                    if not full:
                        P.dma("sp", self.of.rearrange("(h p) t -> p h t", p=128)[:, :, ofo + t * 128: ofo + (t + 1) * 128], o32[b],
                              reads=[("o32", b)], writes=[("of", name, t)])
                        continue
                    if not do_out:
                        continue
                    ps, pk = self.ps_full()
                    for kc in range(KC):
                        P.op("pe", mm(ps, h16[0][:, kc, 2 + hf * 128:2 + (hf + 1) * 128], w_in16[:, kc, 2576:3088], kc == 0, kc == KC - 1),
                             reads=["w_in16", "h16"], writes=[pk])
                    P.op("act", act(gv[0], ps, AF.Gelu_apprx_tanh), reads=[pk], writes=["gv"])
                    o_flat = o32[b].rearrange("p c n -> p (c n)")
                    P.op("act", act(osq16[0], o_flat, AF.Square), reads=[("o32", b)], writes=["osq16"])
                    ps, pk = self.ps_full()
                    P.op("pe", mm(ps, self.ones16, osq16[0]), reads=["osq16", "ones16"], writes=[pk])
                    P.op("act", act(orinv[0], ps, AF.Ln, bias=EPS, scale=1.0 / 128.0), reads=[pk], writes=["orinv"])
                    P.op("act", act(orinv[0], orinv[0], AF.Exp, scale=-0.5), reads=["orinv"], writes=["orinv"])
                    P.op("dve", stt(orinv[0], o_flat, ong, orinv[0], ALU.mult, ALU.mult), reads=[("o32", b), "orinv", "cst"], writes=["orinv"])
                    zk = [("zs", c) for c in range(4)]
                    P.op("pool", tt(cat16[:, 0:4, hsl], orinv[0].rearrange("p (c n) -> p c n", c=4), zs[0][:, :, hsl], ALU.mult),
                         reads=["orinv"] + zk, writes=[("cat16", hf)])
                    P.op("dve", (lambda: lambda e: e.bn_stats(out=st6[0][:, 0:6], in_=gv[0]))(), reads=["gv"], writes=["st6"])
                    P.op("dve", (lambda: lambda e: e.bn_aggr(out=st6[0][:, 6:8], in_=st6[0][:, 0:6]))(), reads=["st6"], writes=["st6"])
                    P.op("act", act(st6[0][:, 7:8], st6[0][:, 7:8], AF.Ln, bias=EPS, scale=1.0), reads=["st6"], writes=["st6"])
                    P.op("act", act(st6[0][:, 7:8], st6[0][:, 7:8], AF.Exp, scale=-0.5), reads=["st6"], writes=["st6"])
                    P.op("dve", ts(gv[0], gv[0], st6[0][:, 6:7], ALU.subtract, st6[0][:, 7:8], ALU.mult), reads=["gv", "st6"], writes=["gv"])
                    P.op("pool", tt(gvn16[0], gv[0], sgug, ALU.mult), reads=["gv", "cst"], writes=["gvn16"])
                    for gi in range(4):
                        pm, km = self.ps_q()
                        P.op("pe", mm(pm, gvn16[0][:, gi * 128:(gi + 1) * 128], self.wsT16[:, l * 512 + gi * 128: l * 512 + (gi + 1) * 128]),
                             reads=["gvn16", "wsT16"], writes=[km])
                        P.op("dve", tt(mixt[0], pm, bsrow[:, gi, :], ALU.add), reads=[km, "cst"], writes=["mixt"])
                        P.op("pool", tt(cat16[:, 4 + gi, hsl], mixt[0], gu[0][:, gi, hsl], ALU.mult),
                             reads=["mixt", ("gu", gi)], writes=[("cat16", hf)])
                if not do_out:
                    continue
                for dc in range(KC):
                    py, ky = self.ps_full()
                    for kc in range(KC):
                        P.op("pe", mm(py[:, 0:TW], w_out16[:, kc, dc * 128:(dc + 1) * 128], cat16[:, kc, :], kc == 0, kc == KC - 1),
                             reads=["w_out16", ("cat16", 0), ("cat16", 1)], writes=[ky])
                    P.op("dve", stt(xn[:, dc, :], py[:, 0:TW], V["g1"][:, dc:dc + 1], xt[sb][:, dc, 2:2 + TW], ALU.mult, ALU.add),
                         reads=[ky, ("xt", sb), ("vec", l, strm)], writes=["xn"])
                P.dma("sp", r3(xo)[:, :, st * TW:(st + 1) * TW], xn, reads=["xn"], writes=[("xdst", name)])

    def mlp_phase(self, l):
        P = self.P
        S, CT, NT = self.S, self.CT, self.NT
        last = (l == 1)
        moe = last
        r3 = lambda ap: ap.rearrange("(k p) t -> p k t", p=128)
        xtile = self.f32("xtile", KC * NT).rearrange("p (k n) -> p k n", k=8)
        acc = self.f32("acc", KC * NT).rearrange("p (k n) -> p k n", k=8)
        h16 = self.b16("h16m", KC * NT).rearrange("p (k n) -> p k n", k=8)
        rinv = self.f32("rinvm", NT)
        NHID = 12
        hid = self.b16("hid", NHID * NT).rearrange("p (c n) -> p c n", c=NHID)
        sq16 = hid[:, 0:KC, :]
        sqk = [("hid", c) for c in range(KC)]
        sil = [self.f32("sil", NT) for _ in range(2)]
        ytmp = sil[1]
        NWB = 2
        w13 = [self.b16("w13", 2 * KC * 512).rearrange("p (j k f) -> p j k f", j=2, k=8) for _ in range(NWB)]
        w2 = [self.b16("w2", NHID * 256).rearrange("p (c n) -> p c n", c=NHID) for _ in range(2)]
        gbc = self.f32("gbc", NT)
        gT = rinv
        rs = self.f32("rs", 64)
        sel = self.cs("sel", rows=8).rearrange("p (r m) -> p r m", r=8)
        T = ALU
        if moe:
            experts = [(self.e13b[ex], self.e2b[ex], 28, [(0, 3), (3, 2), (5, 2)], ("e13b", ex), ("e2b", ex), ex) for ex in range(NE)]
        else:
            experts = [(self.f13b, self.f2b, 22, [(0, 3), (3, 3)], "f13b", "f2b", None)]
        streams = [("x", self.xb, self.outT if last else self.xa, S, 0)]
        if not last:
            streams.append(("c", self.cb, self.ca, CT, 1))
        aks = [("accd", dc) for dc in range(KC)]
        nw13 = 0
        nw2 = 0
        for (name, xi, xo, L, strm) in streams:
            V = self.vec[(l, strm)]
            n = min(NT, L)
            NS = [(s0, min(512, n - s0)) for s0 in range(0, n, 512)]

            def rms_rinv(scale):
                P.op("act", act(sq16[:, :, 0:n], xtile[:, :, 0:n], AF.Square), reads=["xtile"], writes=sqk)
                for (s0, sn) in NS:
                    ps, pk = self.ps_full(0, 3)
                    for kc in range(KC):
                        P.op("pe", mm(ps[:, 0:sn], self.ones16, sq16[:, kc, s0:s0 + sn], kc == 0, kc == KC - 1),
                             reads=sqk + ["ones16"], writes=[pk])
                    P.op("act", act(rinv[:, s0:s0 + sn], ps[:, 0:sn], AF.Ln, bias=EPS, scale=scale), reads=[pk], writes=["rinvm"])
                P.op("act", act(rinv[:, 0:n], rinv[:, 0:n], AF.Exp, scale=-0.5), reads=["rinvm"], writes=["rinvm"])

            for t in range(L // n):
                tsl = slice(t * n, (t + 1) * n)
                P.dma("sp", xtile[:, :, 0:n], r3(xi)[:, :, tsl], reads=[("xdst", name)], writes=["xtile"])
                rms_rinv(1.0 / D)
                for kc in range(KC):
                    P.op("dve", stt(acc[:, kc, 0:n], xtile[:, kc, 0:n], V["gs2"][:, kc:kc + 1], rinv[:, 0:n], T.mult, T.mult),
                         reads=["xtile", "rinvm", ("vec", l, strm)], writes=["acc"] + aks)
                    P.op("act", act(acc[:, kc, 0:n], acc[:, kc, 0:n], AF.Identity, bias=V["sh2"][:, kc:kc + 1], scale=1.0),
                         reads=["acc", ("vec", l, strm)], writes=["acc"])
                P.op("pool", cp(h16[:, :, 0:n], acc[:, :, 0:n]), reads=["acc"], writes=["h16m"])
                if moe:
                    lg, m1, mk1, l2, m2, mk2, em, ssum = (rs[:, 0:8], rs[:, 8:9], rs[:, 16:24], rs[:, 24:32], rs[:, 9:10],
                                                          rs[:, 32:40], rs[:, 40:48], rs[:, 10:11])
                    for st_ in range(n // 128):
                        pr, kr = self.ps_q()
                        for kc in range(KC):
                            P.op("pe", mm(pr[:, 0:8], acc[:, kc, st_ * 128:(st_ + 1) * 128], self.rw32[:, kc * 8:(kc + 1) * 8],
                                          kc == 0, kc == KC - 1), reads=["acc", "rw32"], writes=[kr])
                        R = dict(reads=["rs"], writes=["rs"])
                        P.op("dve", tt(lg, pr[:, 0:8], self.cs("rbrow"), T.add), reads=[kr, "cst", "rs"], writes=["rs"])
                        P.op("dve", red(m1, lg, T.max), **R)
                        P.op("dve", ts(mk1, lg, m1, T.is_equal), **R)
                        P.op("dve", stt(l2, mk1, -1e30, lg, T.mult, T.add), **R)
                        P.op("dve", red(m2, l2, T.max), **R)
                        P.op("dve", ts(mk2, l2, m2, T.is_equal), **R)
                        P.op("dve", tt(mk1, mk1, mk2, T.add), **R)
                        P.op("dve", ts(l2, lg, m1, T.subtract), **R)
                        P.op("act", act(l2, l2, AF.Exp), **R)
                        P.op("dve", tt(em, l2, mk1, T.mult), **R)
                        P.op("dve", red(ssum, em, T.add), **R)
                        P.op("dve", (lambda: lambda e: e.reciprocal(out=ssum, in_=ssum))(), **R)
                        P.op("dve", ts(em, em, ssum, T.mult), **R)
                        pt_, kt_ = self.ps_q()
                        P.op("pe", tr(pt_[0:8, 0:128], em, self.cs("ident")), reads=["rs", "cst"], writes=[kt_])
                        P.op("act", act(gT[0:8, st_ * 128:(st_ + 1) * 128], pt_[0:8, 0:128], AF.Copy), reads=[kt_], writes=["rinvm"])
                blocks = []
                for ei, (e13, e2, nfc, groups, k13, k2, ex) in enumerate(experts):
                    for gi, (b0, nb) in enumerate(groups):
                        for bi in range(b0, b0 + nb):
                            blocks.append((ei, gi, bi, bi == b0, bi == b0 + nb - 1))

                def load13(i):
                    ei, gi, bi, _, _ = blocks[i]
                    e13, k13, nfc_ = experts[ei][0], experts[ei][4], experts[ei][2]
                    fw = min(4, nfc_ - bi * 4) * 128
                    P.dma("sp", w13[(nw13 + i) % NWB][:, :, :, 0:fw], e13[bi][:, :, :, 0:fw], reads=[k13],
                          writes=[("w13", (nw13 + i) % NWB)])

                load13(0)
                first = True
                fcs = []
                for i, (ei, gi, bi, gfirst, glast_) in enumerate(blocks):
                    (e13, e2, nfc, groups, k13, k2, ex) = experts[ei]
                    if i + 1 < len(blocks):
                        load13(i + 1)
                    wb = (nw13 + i) % NWB
                    if gfirst:
                        fcs = []
                        if ex is not None and gi == 0:
                            for (s0, sn) in NS:
                                pg, kg = self.ps_full(0, 3)
                                P.op("pe", mm(pg[:, 0:sn], sel[:, ex, :], gT[0:8, s0:s0 + sn]), reads=["rinvm", "cst"], writes=[kg])
                                P.op("act", act(gbc[:, s0:s0 + sn], pg[:, 0:sn], AF.Copy), reads=[kg], writes=["gbc"])
                    nf = min(4, nfc - bi * 4)
                    for j in range(nf):
                        ci = len(fcs)
                        fcs.append(bi * 4 + j)
                        for (s0, sn) in NS:
                            pa, ka = self.ps_full(3, 4)
                            pb_, kb_ = self.ps_full(3, 4)
                            for kc in range(KC):
                                P.op("pe", mm(pa[:, 0:sn], w13[wb][:, 0, kc, j * 128:(j + 1) * 128], h16[:, kc, s0:s0 + sn],
                                              kc == 0, kc == KC - 1), reads=[("w13", wb), "h16m"], writes=[ka])
                            for kc in range(KC):
                                P.op("pe", mm(pb_[:, 0:sn], w13[wb][:, 1, kc, j * 128:(j + 1) * 128], h16[:, kc, s0:s0 + sn],
                                              kc == 0, kc == KC - 1), reads=[("w13", wb), "h16m"], writes=[kb_])
                            P.op("act", act(sil[0][:, s0:s0 + sn], pa[:, 0:sn], AF.Silu), reads=[ka], writes=[("sil", 0, s0)])
                            P.op("dve", tt(hid[:, ci, s0:s0 + sn], pb_[:, 0:sn], sil[0][:, s0:s0 + sn], T.mult),
                                 reads=[kb_, ("sil", 0, s0)], writes=[("hid", ci)])
                    if not glast_:
                        continue
                    ng = len(fcs)
                    for db in range(4):
                        w2b_ = nw2 % 2
                        nw2 += 1
                        P.dma("sp", w2[w2b_][:, 0:ng, :], e2[db][:, fcs[0]:fcs[0] + ng, :], reads=[k2], writes=[("w2", w2b_)])
                        for dj in range(2):
                            dc = db * 2 + dj
                            ak = ("accd", dc)
                            for (s0, sn) in NS:
                                py, ky = self.ps_full(3, 4)
                                for ci in range(ng):
                                    P.op("pe", mm(py[:, 0:sn], w2[w2b_][:, ci, dj * 128:(dj + 1) * 128], hid[:, ci, s0:s0 + sn],
                                                  ci == 0, ci == ng - 1), reads=[("w2", w2b_), ("hid", ci)], writes=[ky])
                                o_ = acc[:, dc, s0:s0 + sn]
                                if first:
                                    if ex is None:
                                        P.op("act", act(o_, py[:, 0:sn], AF.Copy), reads=[ky], writes=[ak, "acc"])
                                    else:
                                        P.op("dve", tt(o_, py[:, 0:sn], gbc[:, s0:s0 + sn], T.mult), reads=[ky, "gbc"], writes=[ak, "acc"])
                                elif ex is None:
                                    P.op("dve", tt(o_, py[:, 0:sn], o_, T.add), reads=[ky, ak], writes=[ak])
                                else:
                                    P.op("dve", tt(ytmp[:, s0:s0 + sn], py[:, 0:sn], gbc[:, s0:s0 + sn], T.mult),
                                         reads=[ky, "gbc"], writes=[("sil", 1, s0)])
                                    P.op("pool", tt(o_, o_, ytmp[:, s0:s0 + sn], T.add), reads=[("sil", 1, s0), ak], writes=[ak])
                    first = False
                nw13 += len(blocks)
                for dc in range(KC):
                    P.op("dve", stt(xtile[:, dc, 0:n], acc[:, dc, 0:n], V["g2"][:, dc:dc + 1], xtile[:, dc, 0:n], T.mult, T.add),
                         reads=[("accd", dc), "xtile", ("vec", l, strm)], writes=["xtile"])
                if last:
                    rms_rinv(1.0 / D)
                    fg = self.cs("fing")
                    for kc in range(KC):
                        P.op("dve", stt(xtile[:, kc, 0:n], xtile[:, kc, 0:n], fg[:, kc:kc + 1], rinv[:, 0:n], T.mult, T.mult),
                             reads=["xtile", "rinvm", "cst"], writes=["xtile"])
                tok = P.dma("sp", r3(xo)[:, :, tsl], xtile[:, :, 0:n], reads=["xtile"], writes=[("xsrc", name)])
                if last:
                    self.out_toks.append(tok)


_NC_CACHE = {}


def _get_nc(S, CT, debug=False):
    key = (S, CT, debug)
    if key not in _NC_CACHE:
        bld = Builder(S, CT, debug)
        _NC_CACHE[key] = bld.build()
        _NC_CACHE[("bld",) + key] = bld
    return _NC_CACHE[key]


def make_in_maps(inp, nb):
    S = inp["x"].shape[1]
    A = lambda v: np.ascontiguousarray(np.asarray(v, np.float32))
    posT = grid_pos_embed_T(S, D)
    wsT = A(np.transpose(np.asarray(inp["w_s"], np.float32), (0, 3, 1, 2)).reshape(2, 128, 512))
    shared = {
        "posT": posT, "wsT": wsT,
        "ffn_w1": A(inp["ffn_w1"][0]), "ffn_w3": A(inp["ffn_w3"][0]), "ffn_w2": A(inp["ffn_w2"][0]),
        "exp_w1": A(inp["exp_w1"][0]), "exp_w3": A(inp["exp_w3"][0]), "exp_w2": A(inp["exp_w2"][0]),
        "router_w": A(inp["router_w"][0]),
    }
    for l in range(2):
        shared[f"w_mod{l}"] = A(inp["w_mod"][l])
        shared[f"w_in{l}"] = A(inp["w_in"][l])
        shared[f"w_out{l}"] = A(inp["w_out"][l])
    maps = []
    for b in range(nb):
        m = dict(shared)
        m["xT"] = A(np.asarray(inp["x"][b], np.float32).T)
        m["ctxT"] = A(np.asarray(inp["ctx"][b], np.float32).T)
        m["cst"] = build_consts(inp, b)
        maps.append(m)
    return maps


def kernel(**inputs):
    inp = {k: np.asarray(v) for k, v in inputs.items()}
    nb, S, _ = inp["x"].shape
    CT = inp["ctx"].shape[1]
    nc = _get_nc(S, CT)
    maps = make_in_maps(inp, nb)
    res = run_bass_kernel_spmd(nc, maps, core_ids=list(range(nb)))
    out = np.stack([np.ascontiguousarray(r["outT"].T) for r in res.results], axis=0)
    return out.astype(np.float32)
```
